# Optimizing a Trainium2 kernel written in Bass

```python
import math
import jax, jax.numpy as jnp
from jax import lax
import numpy as np

D_MODEL = 2048
BATCH = 4
SEQ = 2048
DEPTH = 2

DN_ALPHA = (2.0 * DEPTH) ** 0.25
DN_BETA = (8.0 * DEPTH) ** -0.25
N_EVEN = (DEPTH + 1) // 2
N_ODD = DEPTH // 2

GDN_HEAD_DIM = 128
GDN_HEADS = D_MODEL // (2 * GDN_HEAD_DIM)
GDN_WIDTH = GDN_HEADS * GDN_HEAD_DIM
GDN_CONV = 4
GDN_CHUNK = 64
SC_WIDTH = D_MODEL - GDN_WIDTH
SC_CONV = 3
HYB_SPLITS = [3 * GDN_WIDTH, 4 * GDN_WIDTH, 4 * GDN_WIDTH + GDN_HEADS,
              4 * GDN_WIDTH + 2 * GDN_HEADS, 4 * GDN_WIDTH + 2 * GDN_HEADS + SC_WIDTH,
              4 * GDN_WIDTH + 2 * GDN_HEADS + 2 * SC_WIDTH]
HYB_IN = 4 * GDN_WIDTH + 2 * GDN_HEADS + 3 * SC_WIDTH
HYB_OUT = GDN_WIDTH + SC_WIDTH

MLA_HEADS = 16
MLA_Q_RANK = 512
MLA_KV_RANK = 512
MLA_NOPE = 128
MLA_ROPE = 64
MLA_V = 128
MLA_IN = MLA_Q_RANK + MLA_KV_RANK + MLA_ROPE
ROPE_THETA = 10000.0
Q_BLOCK = 128
MAX_POS_OFFSET = 4096

MOE_GROUPS = 8
MOE_PER_GROUP = 8
MOE_EXPERTS = MOE_GROUPS * MOE_PER_GROUP
MOE_TOPK = 2
MOE_FF = 512
MOE_BLOCK = 128

kernel_name = "hybrid_gdn_shortconv_mla_hmoe_deepnorm_adaln"

F32 = jnp.float32


def _layer_norm(x, g, b, eps=1e-5):
    xf = x.astype(F32)
    mu = jnp.mean(xf, -1, keepdims=True)
    var = jnp.mean(jnp.square(xf - mu), -1, keepdims=True)
    return ((xf - mu) * lax.rsqrt(var + eps) * g.astype(F32) + b.astype(F32)).astype(x.dtype)


def _rms_norm(x, w, eps=1e-6):
    xf = x.astype(F32)
    return (xf * lax.rsqrt(jnp.mean(xf * xf, -1, keepdims=True) + eps) * w.astype(F32)).astype(x.dtype)


def _l2norm(x, eps=1e-6):
    xf = x.astype(F32)
    return xf * lax.rsqrt(jnp.sum(xf * xf, -1, keepdims=True) + eps)


def _causal_dwconv(x, w):
    k, s = w.shape[0], x.shape[1]
    xp = jnp.pad(x, ((0, 0), (k - 1, 0), (0, 0)))
    y = w[0] * xp[:, 0:s]
    for j in range(1, k):
        y = y + w[j] * xp[:, j:j + s]
    return y


def _gated_delta_rule(q, k, v, g, beta):
    b, s, h, dk = q.shape
    dv = v.shape[-1]
    L = GDN_CHUNK
    n = s // L

    def chunks(t):
        t = t.astype(F32).reshape(b, n, L, h, *t.shape[3:])
        return jnp.moveaxis(t, (1, 3), (0, 2))

    qc = chunks(q) * (dk ** -0.5)
    kc, vc, bc = chunks(k), chunks(v), chunks(beta)
    gc = jnp.cumsum(chunks(g), -1)
    causal = jnp.tril(jnp.ones((L, L), bool))
    strict = jnp.tril(jnp.ones((L, L), bool), -1)
    diff = gc[..., :, None] - gc[..., None, :]
    decay = jnp.where(causal, jnp.exp(jnp.where(causal, diff, 0.0)), 0.0)
    kb = kc * bc[..., None]
    m = jnp.where(strict, jnp.einsum('nbhid,nbhjd->nbhij', kb, kc) * decay, 0.0)
    a = m + jnp.eye(L, dtype=F32)
    rhs = jnp.concatenate([vc * bc[..., None], kb * jnp.exp(gc)[..., None]], -1)
    sol = lax.linalg.triangular_solve(a, rhs, left_side=True, lower=True, unit_diagonal=True)
    u, w = sol[..., :dv], sol[..., dv:]
    qk = jnp.where(causal, jnp.einsum('nbhid,nbhjd->nbhij', qc, kc) * decay, 0.0)
    g_last = gc[..., -1]

    def step(state, inp):
        q_i, k_i, u_i, w_i, qk_i, g_i, gl_i = inp
        v_new = u_i - jnp.einsum('bhld,bhde->bhle', w_i, state)
        o = (jnp.einsum('bhld,bhde->bhle', q_i * jnp.exp(g_i)[..., None], state)
             + jnp.einsum('bhij,bhje->bhie', qk_i, v_new))
        k_dec = k_i * jnp.exp(gl_i[..., None] - g_i)[..., None]
        state = state * jnp.exp(gl_i)[..., None, None] + jnp.einsum('bhld,bhle->bhde', k_dec, v_new)
        return state, o

    state0 = jnp.zeros((b, h, dk, dv), F32)
    _, o = lax.scan(step, state0, (qc, kc, u, w, qk, gc, g_last))
    return jnp.moveaxis(o, (0, 2), (1, 3)).reshape(b, s, h, dv)


def _gdn_shortconv_mixer(h, w_in, conv_w, a_log, dt_bias, norm_w, sc_w, w_out):
    b, s, _ = h.shape
    qkv, z, beta_raw, a_raw, sc_b, sc_c, sc_h = jnp.split(h @ w_in, HYB_SPLITS, -1)
    qkv = jax.nn.silu(_causal_dwconv(qkv, conv_w))
    q, k, v = jnp.split(qkv.reshape(b, s, 3 * GDN_HEADS, GDN_HEAD_DIM), 3, axis=2)
    beta = jax.nn.sigmoid(beta_raw.astype(F32))
    g = -jnp.exp(a_log.astype(F32)) * jax.nn.softplus(a_raw.astype(F32) + dt_bias.astype(F32))
    o = _gated_delta_rule(_l2norm(q), _l2norm(k), v, g, beta)
    o = _rms_norm(o, norm_w) * jax.nn.silu(z.reshape(b, s, GDN_HEADS, GDN_HEAD_DIM).astype(F32))
    y_a = o.reshape(b, s, GDN_WIDTH).astype(h.dtype)
    y_b = sc_b * _causal_dwconv(sc_c * sc_h, sc_w)
    return jnp.concatenate([y_a, y_b], -1) @ w_out


def _rope_tables(pos):
    half = MLA_ROPE // 2
    inv = ROPE_THETA ** (-jnp.arange(half, dtype=F32) * (2.0 / MLA_ROPE))
    ang = pos.astype(F32)[..., None] * inv
    return jnp.cos(ang), jnp.sin(ang)


def _rope(x, cos, sin):
    x1, x2 = jnp.split(x.astype(F32), 2, -1)
    return jnp.concatenate([x1 * cos - x2 * sin, x1 * sin + x2 * cos], -1).astype(x.dtype)


def _mla_attention(q_nope, q_pe, k_nope, k_pe, v):
    b, s, h, _ = q_nope.shape
    nb = s // Q_BLOCK
    scale = (MLA_NOPE + MLA_ROPE) ** -0.5
    kpos = jnp.arange(s)

    def to_blocks(t):
        return jnp.moveaxis(t.reshape(b, nb, Q_BLOCK, *t.shape[2:]), 1, 0)

    def attend(args):
        i, qn, qp = args
        sc = (jnp.einsum('bqhd,bkhd->bhqk', qn, k_nope, preferred_element_type=F32)
              + jnp.einsum('bqhr,bkr->bhqk', qp, k_pe, preferred_element_type=F32)) * scale
        qpos = i * Q_BLOCK + jnp.arange(Q_BLOCK)
        sc = jnp.where(qpos[:, None] >= kpos[None, :], sc, -jnp.inf)
        p = jax.nn.softmax(sc, -1)
        return jnp.einsum('bhqk,bkhd->bqhd', p.astype(v.dtype), v)

    o = lax.map(attend, (jnp.arange(nb), to_blocks(q_nope), to_blocks(q_pe)))
    return jnp.moveaxis(o, 0, 1).reshape(b, s, h, MLA_V)


def _mla_mixer(h, positions, w_in, q_norm, kv_norm, w_uq, w_ukv, w_out):
    b, s, _ = h.shape
    cq, ckv, k_pe = jnp.split(h @ w_in, [MLA_Q_RANK, MLA_Q_RANK + MLA_KV_RANK], -1)
    q = (_rms_norm(cq, q_norm) @ w_uq).reshape(b, s, MLA_HEADS, MLA_NOPE + MLA_ROPE)
    kv = (_rms_norm(ckv, kv_norm) @ w_ukv).reshape(b, s, MLA_HEADS, MLA_NOPE + MLA_V)
    q_nope, q_pe = jnp.split(q, [MLA_NOPE], -1)
    k_nope, v = jnp.split(kv, [MLA_NOPE], -1)
    cos, sin = _rope_tables(positions)
    q_pe = _rope(q_pe, cos[:, :, None], sin[:, :, None])
    k_pe = _rope(k_pe, cos, sin)
    o = _mla_attention(q_nope, q_pe, k_nope, k_pe, v)
    return o.reshape(b, s, MLA_HEADS * MLA_V) @ w_out


def _hier_moe(h, wr_g, br_g, wr_e, br_e, w_gate, w_up, w_down):
    b, s, d = h.shape
    t = b * s
    xt = h.reshape(t, d)
    lg = (xt @ wr_g).astype(F32) + br_g.astype(F32)
    pg = jax.nn.softmax(lg, -1)
    grp = jnp.argmax(lg, -1).astype(jnp.int32)
    pg_sel = jnp.take_along_axis(pg, grp[:, None], -1)
    le = ((xt @ wr_e).astype(F32) + br_e.astype(F32)).reshape(t, MOE_GROUPS, MOE_PER_GROUP)
    le = jnp.take_along_axis(le, grp[:, None, None], 1)[:, 0]
    top_p, top_i = lax.top_k(jax.nn.softmax(le, -1), MOE_TOPK)
    gates = pg_sel * top_p / jnp.sum(top_p, -1, keepdims=True)
    experts = grp[:, None] * MOE_PER_GROUP + top_i.astype(jnp.int32)
    n_assign = t * MOE_TOPK
    e_flat = experts.reshape(-1)
    g_flat = gates.reshape(-1)
    tok_flat = jnp.repeat(jnp.arange(t, dtype=jnp.int32), MOE_TOPK)
    order = jnp.argsort(e_flat)
    e_sorted = e_flat[order]
    counts = jnp.bincount(e_flat, length=MOE_EXPERTS)
    padded = (counts + MOE_BLOCK - 1) // MOE_BLOCK * MOE_BLOCK
    pad_end = jnp.cumsum(padded)
    pad_start = pad_end - padded
    start = jnp.cumsum(counts) - counts
    dest = pad_start[e_sorted] + jnp.arange(n_assign) - start[e_sorted]
    n_rows = n_assign + MOE_EXPERTS * MOE_BLOCK
    n_blocks = n_rows // MOE_BLOCK
    row_tok = jnp.full((n_rows,), t, jnp.int32).at[dest].set(tok_flat[order])
    row_gate = jnp.zeros((n_rows,), F32).at[dest].set(g_flat[order])
    block_expert = jnp.minimum(
        jnp.searchsorted(pad_end, jnp.arange(n_blocks) * MOE_BLOCK, side='right'), MOE_EXPERTS - 1)
    x_pad = jnp.concatenate([xt, jnp.zeros((1, d), xt.dtype)], 0)
    xs = x_pad[row_tok].reshape(n_blocks, MOE_BLOCK, d)

    def expert_block(args):
        e, xb = args
        hid = jax.nn.silu(xb @ w_gate[e]) * (xb @ w_up[e])
        return hid @ w_down[e]

    ys = lax.map(expert_block, (block_expert, xs)).reshape(n_rows, d)
    ys = ys * row_gate[:, None].astype(ys.dtype)
    out = jax.ops.segment_sum(ys, row_tok, num_segments=t + 1)[:t]
    return out.reshape(b, s, d)


def setup_inputs(seed: int = 0) -> dict:
    key = jax.random.key(seed)
    ks = iter(jax.random.split(key, 48))

    def nrm(shape, std):
        return jax.random.normal(next(ks), shape, F32) * std

    ne, no, D = N_EVEN, N_ODD, D_MODEL
    x = nrm((BATCH, SEQ, D), 1.0)
    c = nrm((BATCH, D), 1.0)
    positions = (jax.random.randint(next(ks), (BATCH, 1), 0, MAX_POS_OFFSET, jnp.int32)
                 + jnp.arange(SEQ, dtype=jnp.int32)[None, :])
    ada_w = nrm((DEPTH, D, 6 * D), 0.1 * D ** -0.5)
    ada_b = nrm((DEPTH, 6 * D), 0.01)
    ln_g = 1.0 + nrm((DEPTH, 2, D), 0.02)
    ln_b = nrm((DEPTH, 2, D), 0.02)
    hyb_w_in = nrm((ne, D, HYB_IN), D ** -0.5)
    gdn_conv_w = nrm((ne, GDN_CONV, 3 * GDN_WIDTH), GDN_CONV ** -0.5)
    gdn_a_log = jnp.log(jax.random.uniform(next(ks), (ne, GDN_HEADS), F32, 1.0, 16.0))
    dt = jnp.exp(jax.random.uniform(next(ks), (ne, GDN_HEADS), F32, math.log(1e-3), math.log(1e-1)))
    gdn_dt_bias = dt + jnp.log(-jnp.expm1(-dt))
    gdn_norm_w = 1.0 + nrm((ne, GDN_HEAD_DIM), 0.02)
    sc_conv_w = nrm((ne, SC_CONV, SC_WIDTH), SC_CONV ** -0.5)
    hyb_w_out = nrm((ne, HYB_OUT, D), DN_BETA * HYB_OUT ** -0.5)
    mla_w_in = nrm((no, D, MLA_IN), D ** -0.5)
    mla_q_norm = 1.0 + nrm((no, MLA_Q_RANK), 0.02)
    mla_kv_norm = 1.0 + nrm((no, MLA_KV_RANK), 0.02)
    mla_w_uq = nrm((no, MLA_Q_RANK, MLA_HEADS * (MLA_NOPE + MLA_ROPE)), MLA_Q_RANK ** -0.5)
    mla_w_ukv = nrm((no, MLA_KV_RANK, MLA_HEADS * (MLA_NOPE + MLA_V)), MLA_KV_RANK ** -0.5)
    mla_w_out = nrm((no, MLA_HEADS * MLA_V, D), DN_BETA * (MLA_HEADS * MLA_V) ** -0.5)
    moe_router_g = nrm((DEPTH, D, MOE_GROUPS), D ** -0.5)
    moe_bias_g = nrm((DEPTH, MOE_GROUPS), 0.01)
    moe_router_e = nrm((DEPTH, D, MOE_EXPERTS), D ** -0.5)
    moe_bias_e = nrm((DEPTH, MOE_EXPERTS), 0.01)
    moe_w_gate = nrm((DEPTH, MOE_EXPERTS, D, MOE_FF), D ** -0.5)
    moe_w_up = nrm((DEPTH, MOE_EXPERTS, D, MOE_FF), D ** -0.5)
    moe_w_down = nrm((DEPTH, MOE_EXPERTS, MOE_FF, D), DN_BETA * MOE_FF ** -0.5)
    return {"x": x, "c": c, "positions": positions, "ada_w": ada_w, "ada_b": ada_b,
            "ln_g": ln_g, "ln_b": ln_b, "hyb_w_in": hyb_w_in, "gdn_conv_w": gdn_conv_w,
            "gdn_a_log": gdn_a_log, "gdn_dt_bias": gdn_dt_bias, "gdn_norm_w": gdn_norm_w,
            "sc_conv_w": sc_conv_w, "hyb_w_out": hyb_w_out, "mla_w_in": mla_w_in,
            "mla_q_norm": mla_q_norm, "mla_kv_norm": mla_kv_norm, "mla_w_uq": mla_w_uq,
            "mla_w_ukv": mla_w_ukv, "mla_w_out": mla_w_out, "moe_router_g": moe_router_g,
            "moe_bias_g": moe_bias_g, "moe_router_e": moe_router_e, "moe_bias_e": moe_bias_e,
            "moe_w_gate": moe_w_gate, "moe_w_up": moe_w_up, "moe_w_down": moe_w_down}


def reference(x, c, positions, ada_w, ada_b, ln_g, ln_b, hyb_w_in, gdn_conv_w, gdn_a_log,
              gdn_dt_bias, gdn_norm_w, sc_conv_w, hyb_w_out, mla_w_in, mla_q_norm, mla_kv_norm,
              mla_w_uq, mla_w_ukv, mla_w_out, moe_router_g, moe_bias_g, moe_router_e, moe_bias_e,
              moe_w_gate, moe_w_up, moe_w_down):
    c_act = jax.nn.silu(c)
    for layer in range(DEPTH):
        mod = (c_act @ ada_w[layer] + ada_b[layer])[:, None, :]
        sh1, sc1, g1, sh2, sc2, g2 = jnp.split(mod, 6, -1)
        hin = x * (1.0 + sc1) + sh1
        i = layer // 2
        if layer % 2 == 0:
            y = _gdn_shortconv_mixer(hin, hyb_w_in[i], gdn_conv_w[i], gdn_a_log[i], gdn_dt_bias[i],
                                     gdn_norm_w[i], sc_conv_w[i], hyb_w_out[i])
        else:
            y = _mla_mixer(hin, positions, mla_w_in[i], mla_q_norm[i], mla_kv_norm[i],
                           mla_w_uq[i], mla_w_ukv[i], mla_w_out[i])
        x = _layer_norm(DN_ALPHA * x + (1.0 + g1) * y, ln_g[layer, 0], ln_b[layer, 0])
        hin = x * (1.0 + sc2) + sh2
        y = _hier_moe(hin, moe_router_g[layer], moe_bias_g[layer], moe_router_e[layer],
                      moe_bias_e[layer], moe_w_gate[layer], moe_w_up[layer], moe_w_down[layer])
        x = _layer_norm(DN_ALPHA * x + (1.0 + g2) * y, ln_g[layer, 1], ln_b[layer, 1])
    return x
```

```python
from contextlib import ExitStack
import numpy as np
import concourse.bass as bass
import concourse.mybir as mybir
from concourse.bass_utils import run_bass_kernel_spmd

F32 = mybir.dt.float32
BF16 = mybir.dt.bfloat16
I32 = mybir.dt.int32
AF = mybir.ActivationFunctionType
ALU = mybir.AluOpType
AX = mybir.AxisListType
ENGS = ("sync", "scalar", "vector", "gpsimd", "tensor")
_DTSZ = {"float32": 4, "bfloat16": 2, "int32": 4, "float16": 2, "uint32": 4, "int16": 2,
         "uint16": 2, "int8": 1, "uint8": 1, "float32r": 4}
WRITE_KW = ("out", "accum_out", "out_max", "out_indices", "ap")


def _dtsz(dt):
    s = str(dt).split(".")[-1]
    return _DTSZ.get(s, 4)


def _rect(ap):
    name = ap.tensor.name
    pat = ap.ap
    off = ap.offset
    esz = _dtsz(ap.dtype)
    space = str(ap.space)
    if not isinstance(off, int):
        return (name, 0, 1 << 30, 0, 1 << 40)
    if "DRAM" in space or "Dram" in space:
        ext = sum((c - 1) * abs(s) for s, c in pat) + 1
        return (name, 0, 1, off * esz, (off + ext) * esz)
    if "PSUM" in space.upper():
        return (name, 0, 128, 0, 1 << 20)
    ps, pc = pat[0]
    if ps == 0:
        ps = 1 << 30
    p0 = off // ps if ps < (1 << 30) else 0
    fo = off - p0 * ps if ps < (1 << 30) else off
    ext = sum((c - 1) * abs(s) for s, c in pat[1:]) + 1
    return (name, p0, p0 + pc, fo * esz, (fo + ext) * esz)


def _ovl(a, b):
    return a[1] < b[2] and b[1] < a[2] and a[3] < b[4] and b[3] < a[4]


def _contains(a, b):
    return a[1] <= b[1] and b[2] <= a[2] and a[3] <= b[3] and b[4] <= a[4]


class Prog:
    def __init__(self, nc, serialize=False):
        self.nc = nc
        self.es = ExitStack()
        self.ops = {e: [] for e in ENGS}
        self.cnt = {e: 0 for e in ENGS}
        self.esem = {}
        self.slot_cnt = {}
        self.slot_sem = {}
        self.slot_waiters = {}
        self.recs = {}
        self.waited = {e: {} for e in ENGS}
        self.serialize = serialize
        self.last_event = None
        self.nuid = 0
        for e in ENGS:
            self.esem[e] = self.es.enter_context(nc.semaphore("es_" + e))

    def sb(self, name, shape, dt=F32):
        return self.es.enter_context(self.nc.sbuf_tensor(name, list(shape), dt))

    def ps(self, name, shape, dt=F32):
        return self.es.enter_context(self.nc.psum_tensor(name, list(shape), dt))

    def dram(self, name, shape, dt=F32, kind="Internal"):
        return self.nc.dram_tensor(name, list(shape), dt, kind=kind).ap()

    def _slot_sem(self, slot):
        if slot not in self.slot_sem:
            self.slot_sem[slot] = self.es.enter_context(self.nc.semaphore("ds_%d" % len(self.slot_sem)))
            self.slot_cnt[slot] = 0
            self.slot_waiters[slot] = {}
        return self.slot_sem[slot]

    def _deps(self, reads, writes, eng, is_pe):
        deps = []
        for ap in reads:
            r = _rect(ap)
            psum = r[4] == (1 << 20)
            for (rr, kind, ev, e2) in self.recs.get(r[0], []):
                if _ovl(r, rr) and (kind == "w" or (psum and e2 != eng)):
                    deps.append((ev, e2))
        for ap in writes:
            r = _rect(ap)
            for (rr, kind, ev, e2) in self.recs.get(r[0], []):
                if _ovl(r, rr):
                    deps.append((ev, e2))
        return deps

    def _record(self, reads, writes, ev, eng):
        for ap in writes:
            r = _rect(ap)
            lst = self.recs.setdefault(r[0], [])
            lst[:] = [x for x in lst if not _contains(r, x[0])]
            lst.append((r, "w", ev, eng))
        for ap in reads:
            r = _rect(ap)
            lst = self.recs.setdefault(r[0], [])
            lst[:] = [x for x in lst if not (x[1] == "r" and x[3] == eng and x[0] == r
                                             and x[2][0] == ev[0])]
            lst.append((r, "r", ev, eng))

    def _mkwaits(self, eng, deps, is_pe):
        waits = {}
        for (ev, e2) in deps:
            if ev is None:
                continue
            key, val = ev
            if is_pe and key == ("e", "tensor"):
                continue
            if key[0] == "d":
                val = self.slot_cnt[key[1]]
            if self.waited[eng].get(key, 0) >= val:
                continue
            waits[key] = max(waits.get(key, 0), val)
        for key, val in waits.items():
            self.waited[eng][key] = val
        return waits

    def op(self, eng, method, *args, reads=(), writes=(), **kw):
        aps_r = list(reads)
        aps_w = list(writes)
        for k, v in kw.items():
            if hasattr(v, "tensor") and hasattr(v, "ap"):
                (aps_w if k in WRITE_KW else aps_r).append(v)
        for v in args:
            if hasattr(v, "tensor") and hasattr(v, "ap"):
                aps_r.append(v)
        is_pe = eng == "tensor"
        deps = self._deps(aps_r, aps_w, eng, is_pe)
        if self.serialize and self.last_event is not None:
            deps.append((self.last_event, None))
        waits = self._mkwaits(eng, deps, is_pe)
        self.cnt[eng] += 1
        ev = (("e", eng), self.cnt[eng])
        for key in waits:
            if key[0] == "d":
                self.slot_waiters[key[1]][eng] = ev
        self._record(aps_r, aps_w, ev, eng)
        self.ops[eng].append(("c", method, args, kw, waits, ev))
        self.last_event = ev
        return ev

    def dma(self, eng, out, in_, slot=None, indirect=None, **kw):
        if slot is None:
            slot = out.tensor.name
        if eng == "gpsimd":
            slot = "sw_" + slot
        self._slot_sem(slot)
        aps_r = [in_]
        aps_w = [out]
        if indirect is not None:
            aps_r.append(indirect["idx"])
        deps = self._deps(aps_r, aps_w, eng, False)
        for e2, ev2 in self.slot_waiters[slot].items():
            deps.append((ev2, e2))
        if self.serialize and self.last_event is not None:
            deps.append((self.last_event, None))
        waits = self._mkwaits(eng, deps, False)
        self.slot_cnt[slot] += 16
        ev = (("d", slot), self.slot_cnt[slot])
        for key in waits:
            if key[0] == "d" and key[1] != slot:
                self.slot_waiters[key[1]][eng + "_q"] = ev
        self._record(aps_r, aps_w, ev, "dma_" + eng)
        self.ops[eng].append(("d", None, (out, in_, indirect), kw, waits, ev))
        self.last_event = ev
        return ev

    def dyn(self, eng, fn, reads=(), writes=(), slot=None):
        if eng == "gpsimd":
            slot = "sw_" + slot
        self._slot_sem(slot)
        deps = self._deps(list(reads), list(writes), eng, False)
        for e2, ev2 in self.slot_waiters[slot].items():
            deps.append((ev2, e2))
        waits = self._mkwaits(eng, deps, False)
        self.slot_cnt[slot] += 16
        ev = (("d", slot), self.slot_cnt[slot])
        for key in waits:
            if key[0] == "d" and key[1] != slot:
                self.slot_waiters[key[1]][eng + "_q"] = ev
        self._record(list(reads), list(writes), ev, "dma_" + eng)
        self.ops[eng].append(("f", fn, None, None, waits, ev))
        return ev

    def _sem_of(self, key):
        return self.esem[key[1]] if key[0] == "e" else self.slot_sem[key[1]]

    def _emit(self, engname, e):
        for (kind, method, args, kw, waits, ev) in self.ops[engname]:
            for key, val in waits.items():
                e.wait_ge(self._sem_of(key), val)
            if kind == "c":
                ins = getattr(e, method)(*args, **kw)
                ins.then_inc(self.esem[engname], 1)
            elif kind == "f":
                ins = method(e)
                ins.then_inc(self._sem_of(ev[0]), 16)
            else:
                out, in_, ind = args
                if ind is None:
                    ins = e.dma_start(out=out, in_=in_, **kw)
                else:
                    off = bass.IndirectOffsetOnAxis(ap=ind["idx"], axis=0)
                    if ind["mode"] == "gather":
                        ins = e.indirect_dma_start(out=out, out_offset=None, in_=in_, in_offset=off, **kw)
                    else:
                        ins = e.indirect_dma_start(out=out, out_offset=off, in_=in_, in_offset=None, **kw)
                ins.then_inc(self._sem_of(ev[0]), 16)
        if engname == "sync":
            for slot, c in self.slot_cnt.items():
                if c:
                    e.wait_ge(self.slot_sem[slot], c)
            for en in ENGS:
                if en != "sync" and self.cnt[en]:
                    e.wait_ge(self.esem[en], self.cnt[en])

    def finish(self):
        nc = self.nc
        with nc.Block() as block:
            @block.sync
            def _(e):
                self._emit("sync", e)

            @block.scalar
            def _(e):
                self._emit("scalar", e)

            @block.vector
            def _(e):
                self._emit("vector", e)

            @block.gpsimd
            def _(e):
                self._emit("gpsimd", e)

            @block.tensor
            def _(e):
                self._emit("tensor", e)
        self.es.close()
        return nc


def build_modA():
    nc = bass.Bass("TRN2", target_bir_lowering=False)
    P = Prog(nc)
    cT = nc.dram_tensor("cT", [128, 16, 4], F32, kind="ExternalInput").ap()
    aw = nc.dram_tensor("aw", [2, 2048, 1536], F32, kind="ExternalInput").ap()
    ab = nc.dram_tensor("ab", [2, 1536], F32, kind="ExternalInput").ap()
    out = nc.dram_tensor("mod", [2, 4, 1536], F32, kind="ExternalOutput").ap()
    ct = P.sb("ct", [128, 64]); cs = P.sb("cs", [128, 64])
    wb = [P.sb("w%d" % i, [128, 4, 1536]) for i in range(2)]
    bias = P.sb("bias", [4, 2, 1536]); res = P.sb("res", [4, 2, 1536])
    pp = [P.ps("pp%d" % i, [128, 2048]) for i in range(2)]
    P.dma("sync", out=ct[:], in_=cT.rearrange("p k b -> p (k b)"))
    for l in range(2):
        for b in range(4):
            P.dma("sync", out=bias[b:b+1, l, :], in_=ab[l:l+1, :])
    P.op("scalar", "activation", out=cs[:], in_=ct[:], func=AF.Silu)
    it = 0
    for l in range(2):
        for kg in range(4):
            w = wb[it % 2]; it += 1
            P.dma("sync", out=w[:], in_=aw[l, kg*512:(kg+1)*512, :].rearrange("(k p) n -> p k n", p=128))
            for kk in range(4):
                k = kg*4+kk
                for j in range(3):
                    P.op("tensor", "matmul", out=pp[l][0:4, j*512:(j+1)*512], lhsT=cs[:, k*4:(k+1)*4],
                         rhs=w[:, kk, j*512:(j+1)*512], start=(k == 0), stop=(k == 15))
        P.op("vector", "tensor_tensor", out=res[:, l, :], in0=pp[l][0:4, 0:1536], in1=bias[:, l, :], op=ALU.add)
        P.dma("sync", out=out[l], in_=res[:, l, :])
    return P.finish()


import numpy as np

NT = 16


def consts_np():
    i = np.arange(128)
    ident = np.eye(128, dtype=np.float32)
    U = (i[:, None] <= i[None, :]).astype(np.float32)
    negmask = -(i[:, None] > i[None, :]).astype(np.float32)
    maskT = (i[:, None] <= i[None, :]).astype(np.float32)
    ones = np.ones((128, 128), np.float32)
    return np.ascontiguousarray(np.concatenate([ident, U, negmask, maskT, ones], axis=1))


def build_B(stop=None):
    nc = bass.Bass("TRN2", target_bir_lowering=False)
    import os
    P = Prog(nc, serialize=bool(os.environ.get("SER")))
    xT = nc.dram_tensor("xT", [128, 16, 2048], F32, kind="ExternalInput").ap()
    modc = nc.dram_tensor("modc", [128, 32], F32, kind="ExternalInput").ap()
    wf = nc.dram_tensor("wf", [8, 2048, 384], F32, kind="ExternalInput").ap()
    wz = nc.dram_tensor("wz", [2048, 512], F32, kind="ExternalInput").ap()
    wba = nc.dram_tensor("wba", [128, 16, 8], F32, kind="ExternalInput").ap()
    cw = nc.dram_tensor("cw", [128, 12, 4], F32, kind="ExternalInput").ap()
    scw = nc.dram_tensor("scw", [128, 4, 3], F32, kind="ExternalInput").ap()
    hv = nc.dram_tensor("hv", [128, 8], F32, kind="ExternalInput").ap()
    nw = nc.dram_tensor("nw", [128, 128], F32, kind="ExternalInput").ap()
    cst = nc.dram_tensor("cst", [128, 640], F32, kind="ExternalInput").ap()
    ya = nc.dram_tensor("ya", [2048, 512], F32, kind="ExternalOutput").ap()
    ybT = nc.dram_tensor("ybT", [512, 2048], F32, kind="ExternalOutput").ap()
    raw = P.dram("rawscr", [24, 128, 2048], F32)

    csb = P.sb("csb", [128, 640])
    ident, U, negmask, maskT, ones = (csb[:, i*128:(i+1)*128] for i in range(5))
    small = P.sb("small", [128, 512])
    mod_sb = small[:, 0:32]; scp1 = small[:, 32:48]
    hv_sb = small[:, 48:56]; ea = small[:, 56:60]
    cw_sb = P.sb("cw_sb", [128, 48]); scw_sb = P.sb("scw_sb", [128, 12])
    nw_sb = P.sb("nw_sb", [128, 128])
    wba_sb = P.sb("wba_sb", [128, 128])
    ba = P.sb("ba", [128, NT * 8])
    g_all = P.sb("g_all", [128, NT * 4]); beta_all = P.sb("beta_all", [128, NT * 4])
    gc_all = P.sb("gc_all", [128, NT * 4])
    sz = P.sb("sz", [128, NT * 512], BF16)
    arena = P.sb("arena", [128, 36 * 1024])
    psb = [P.ps("ps%d" % i, [128, 512]) for i in range(8)]
    pq = [0]

    def psq():
        i = pq[0]; pq[0] = (i + 1) % 32
        return psb[i // 4][:, (i % 4) * 128:(i % 4 + 1) * 128]
    pb = [0]

    def psbank():
        i = pb[0]; pb[0] = (i + 1) % 8
        pq[0] = 0
        return psb[i]

    cb = P.sb("cb", [128, 4])
    P.op("vector", "memset", ap=cb[:, 0:1], constant=1e-6)
    P.op("vector", "memset", ap=cb[:, 1:2], constant=1.0)
    P.dma("sync", out=csb[:], in_=cst)
    P.dma("sync", out=mod_sb, in_=modc)
    P.dma("sync", out=hv_sb, in_=hv)
    P.dma("sync", out=cw_sb[:], in_=cw.rearrange("p a b -> p (a b)"))
    P.dma("sync", out=scw_sb[:], in_=scw.rearrange("p a b -> p (a b)"))
    P.dma("sync", out=nw_sb[:], in_=nw)
    P.dma("sync", out=wba_sb[:], in_=wba.rearrange("p a b -> p (a b)"))
    P.op("vector", "tensor_scalar", out=scp1, in0=mod_sb[:, 0:16], scalar1=1.0, scalar2=None, op0=ALU.add)
    P.op("scalar", "activation", out=ea, in_=hv_sb[:, 0:4], func=AF.Exp)

    hinT = arena[:, 0:16384].bitcast(BF16).rearrange("p (k t) -> p k t", k=16)
    xt = [arena[:, 16384 + i*4096:16384 + (i+1)*4096].rearrange("p (k t) -> p k t", k=16) for i in range(1)]
    hf = arena[:, 20480:24576].rearrange("p (k t) -> p k t", k=16)
    wbuf = [arena[:, 24576 + i*3072:24576 + (i+1)*3072].bitcast(BF16).rearrange("p (k n) -> p k n", k=16)
            for i in range(1)]
    wzb = arena[:, 24576 + 3072:24576 + 3072 + 4096].bitcast(BF16).rearrange("p (k n) -> p k n", k=16)
    ev = [small[:, 64:64], ]
    evb = P.sb("evb", [128, 2 * 512])
    for tt in range(8):
        t0 = tt * 256
        P.dma("sync", out=xt[0], in_=xT[:, :, t0:t0 + 256])
        for k in range(16):
            P.op("scalar", "activation", out=hf[:, k, :], in_=xt[0][:, k, :], func=AF.Identity,
                 scale=scp1[:, k:k+1], bias=mod_sb[:, 16 + k:17 + k])
            P.op("vector" if k % 2 else "gpsimd", "tensor_copy", out=hinT[:, k, t0:t0 + 256], in_=hf[:, k, :])
        for sub in range(2):
            n = tt * 2 + sub
            pp = psbank()
            for k in range(16):
                P.op("tensor", "matmul", out=pp[:, 0:8], lhsT=hf[:, k, sub*128:(sub+1)*128],
                     rhs=wba_sb[:, k*8:(k+1)*8], start=(k == 0), stop=(k == 15))
            P.op("vector", "tensor_copy", out=ba[:, n*8:(n+1)*8], in_=pp[:, 0:8])
    ba3 = ba[:, :].rearrange("p (n c) -> p n c", c=8)
    b3 = beta_all[:, :].rearrange("p (n c) -> p n c", c=4)
    g3 = g_all[:, :].rearrange("p (n c) -> p n c", c=4)
    P.op("scalar", "activation", out=b3, in_=ba3[:, :, 0:4], func=AF.Sigmoid)
    for n in range(NT):
        P.op("vector", "tensor_tensor", out=g3[:, n, :], in0=ba3[:, n, 4:8], in1=hv_sb[:, 4:8], op=ALU.add)
    P.op("scalar", "activation", out=g_all[:], in_=g_all[:], func=AF.Exp)
    P.op("scalar", "activation", out=g_all[:], in_=g_all[:], func=AF.Ln, bias=cb[:, 1:2])
    for n in range(NT):
        P.op("vector", "scalar_tensor_tensor", out=g3[:, n, :], in0=g3[:, n, :], scalar=-1.0, in1=ea,
             op0=ALU.mult, op1=ALU.mult)
    pp = psbank()
    P.op("tensor", "matmul", out=pp[:, 0:64], lhsT=U, rhs=g_all[:], start=True, stop=True)
    P.op("vector", "tensor_copy", out=gc_all[:], in_=pp[:, 0:64])

    if stop == "P0":
        return P.finish()
    P.dma("gpsimd", out=wzb, in_=wz.rearrange("(k p) n -> p k n", p=128))
    for n in range(NT):
        pp = psbank()
        for k in range(16):
            P.op("tensor", "matmul", out=pp[:, :], lhsT=hinT[:, k, n*128:(n+1)*128], rhs=wzb[:, k, :],
                 start=(k == 0), stop=(k == 15))
        P.op("scalar", "activation", out=sz[:, n*512:(n+1)*512], in_=pp[:, :], func=AF.Silu)
    for gi in range(8):
        P.dma("gpsimd", out=wbuf[0], in_=wf[gi].rearrange("(k p) n -> p k n", p=128))
        for c3 in range(3):
            for t4 in range(4):
                pp = psbank()
                for k in range(16):
                    P.op("tensor", "matmul", out=pp[:, :], lhsT=wbuf[0][:, k, c3*128:(c3+1)*128],
                         rhs=hinT[:, k, t4*512:(t4+1)*512], start=(k == 0), stop=(k == 15))
                e = evb[:, (t4 % 2)*512:(t4 % 2 + 1)*512]
                P.op("vector" if t4 % 2 else "scalar", "tensor_copy" if t4 % 2 else "copy", out=e, in_=pp[:, :])
                P.dma("sync", out=raw[gi*3 + c3, :, t4*512:(t4+1)*512], in_=e, slot="rawst%d" % (t4 % 2))

    if stop == "P1":
        return P.finish()
    A = arena
    rawb = A[:, 0:3 * 2052].rearrange("p (c t) -> p c t", c=3)
    QKV = A[:, 6400:6400 + 3 * 2048].rearrange("p (c t) -> p c t", c=3)
    o0 = 12544
    u_all = A[:, o0:o0 + 2048].rearrange("p (n e) -> p n e", n=NT); o0 += 2048
    wT_all = A[:, o0:o0 + 2048]; o0 += 2048
    qkT_all = A[:, o0:o0 + 2048].rearrange("p (n e) -> p n e", n=NT); o0 += 2048
    QgT_all = A[:, o0:o0 + 2048]; o0 += 2048
    kdec_all = A[:, o0:o0 + 2048].rearrange("p (n e) -> p n e", n=NT); o0 += 2048
    ya_h = A[:, o0:o0 + 2048].rearrange("p (n e) -> p n e", n=NT); o0 += 2048
    nz = A[:, o0:o0 + 2048].rearrange("p (n e) -> p n e", n=NT); o0 += 2048
    tmpc = A[:, o0:o0 + 2052]; o0 += 2052
    sq = A[:, o0:o0 + 512]; o0 += 512
    rs = A[:, o0:o0 + 512]; o0 += 512
    egl_all = A[:, o0:o0 + 16]; o0 += 16
    Sst = [A[:, o0 + i*128:o0 + (i+1)*128] for i in range(2)]; o0 += 256
    NSL = 2
    W = []
    for s in range(NSL):
        names = ["gb", "t1", "D1", "t2", "D2", "Egb", "cols", "Nm", "NTm", "Pa", "Pb", "PTa", "PTb", "XTa", "XTb",
                 "vb", "kbg", "t3", "t4"]
        d = {}
        for nm in names:
            d[nm] = A[:, o0:o0 + 128]; o0 += 128
        W.append(d)
    assert o0 <= 36 * 1024, o0
    P.op("vector", "memset", ap=rawb[:, :, 0:4], constant=0.0)

    def phase1_steps(h, n, s):
        w = W[s]; tok = slice(n*128, (n+1)*128); col = n*4 + h
        gcol = g_all[:, col:col+1]; gccol = gc_all[:, col:col+1]; bcol = beta_all[:, col:col+1]
        QT = QKV[:, 0, tok]; KT = QKV[:, 1, tok]; VT = QKV[:, 2, tok]
        st = {}

        def s1():
            P.op("vector", "tensor_scalar", out=w["gb"], in0=ones, scalar1=gcol, scalar2=None, op0=ALU.mult)
            st["G"] = psq()
            P.op("tensor", "matmul", out=st["G"], lhsT=w["gb"], rhs=U, start=True, stop=True)
            st["KK"] = psq(); st["QK"] = psq()
            P.op("tensor", "matmul", out=st["KK"], lhsT=KT, rhs=KT, start=True, stop=True)
            P.op("tensor", "matmul", out=st["QK"], lhsT=KT, rhs=QT, start=True, stop=True)

        def s2():
            G = st["G"]
            P.op("vector", "tensor_scalar", out=w["t1"], in0=G, scalar1=gccol, scalar2=0.0, op0=ALU.subtract, op1=ALU.max)
            P.op("scalar", "activation", out=w["D1"], in_=w["t1"], func=AF.Exp, scale=-1.0)
            P.op("vector", "tensor_scalar", out=w["t2"], in0=G, scalar1=gccol, scalar2=0.0, op0=ALU.subtract, op1=ALU.min)
            P.op("scalar", "activation", out=w["D2"], in_=w["t2"], func=AF.Exp)
            P.op("scalar", "activation", out=w["Egb"], in_=G, func=AF.Exp)
            c = w["cols"]
            P.op("scalar", "activation", out=c[:, 0:1], in_=gccol, func=AF.Exp)
            P.op("vector", "tensor_tensor", out=c[:, 1:2], in0=c[:, 0:1], in1=bcol, op=ALU.mult)
            P.op("vector", "tensor_copy", out=c[:, 3:4], in_=G[:, 127:128])
            P.op("vector", "tensor_tensor", out=c[:, 4:5], in0=c[:, 3:4], in1=gccol, op=ALU.subtract)
            P.op("scalar", "activation", out=c[:, 2:3], in_=c[:, 4:5], func=AF.Exp)
            P.op("vector", "tensor_copy", out=egl_all[:, n:n+1], in_=w["Egb"][:, 127:128])

        def s3():
            P.op("vector", "scalar_tensor_tensor", out=w["t3"], in0=st["KK"], scalar=bcol, in1=w["D1"],
                 op0=ALU.mult, op1=ALU.mult)
            P.op("gpsimd", "tensor_tensor", out=w["Nm"], in0=w["t3"], in1=negmask, op=ALU.mult)
            P.op("vector", "tensor_tensor", out=w["t4"], in0=st["QK"], in1=w["D2"], op=ALU.mult)
            P.op("gpsimd", "tensor_tensor", out=qkT_all[:, n, :], in0=w["t4"], in1=maskT, op=ALU.mult)
            P.op("gpsimd", "tensor_tensor", out=QgT_all[:, tok], in0=QT, in1=w["Egb"], op=ALU.mult)
            st["NTp"] = psq()
            P.op("tensor", "transpose", out=st["NTp"], in_=w["Nm"], identity=ident)
            st["Kt"] = psq(); st["Vt"] = psq()
            P.op("tensor", "transpose", out=st["Kt"], in_=KT, identity=ident)
            P.op("tensor", "transpose", out=st["Vt"], in_=VT, identity=ident)

        def s4():
            P.op("scalar", "copy", out=w["NTm"], in_=st["NTp"])
            P.op("vector", "tensor_tensor", out=w["XTa"], in0=st["NTp"], in1=ident, op=ALU.add)
            c = w["cols"]
            P.op("scalar", "activation", out=w["kbg"], in_=st["Kt"], func=AF.Identity, scale=c[:, 1:2])
            P.op("vector", "tensor_scalar", out=kdec_all[:, n, :], in0=st["Kt"], scalar1=c[:, 2:3], scalar2=None, op0=ALU.mult)
            P.op("scalar", "activation", out=w["vb"], in_=st["Vt"], func=AF.Identity, scale=bcol)
            st["P"] = w["Nm"]; st["PT"] = w["NTm"]; st["XT"] = w["XTa"]; st["lvl"] = 0

        def sq_a():
            lvl = st["lvl"]
            st["Pp"] = psq()
            P.op("tensor", "matmul", out=st["Pp"], lhsT=st["PT"], rhs=st["P"], start=True, stop=True)
            if lvl < 5:
                st["PTp"] = psq()
                P.op("tensor", "matmul", out=st["PTp"], lhsT=st["P"], rhs=st["PT"], start=True, stop=True)

        def sq_b():
            lvl = st["lvl"]
            Pn = w["Pa"] if lvl % 2 == 0 else w["Pb"]
            PTn = w["PTa"] if lvl % 2 == 0 else w["PTb"]
            P.op("scalar", "copy", out=Pn, in_=st["Pp"])
            if lvl < 5:
                P.op("vector", "tensor_copy", out=PTn, in_=st["PTp"])
            st["Xp"] = psq()
            P.op("tensor", "matmul", out=st["Xp"], lhsT=Pn, rhs=st["XT"], start=True, stop=True)
            st["P"] = Pn; st["PT"] = PTn

        def sq_c():
            Xn = w["XTb"] if st["XT"] is w["XTa"] else w["XTa"]
            P.op("vector", "tensor_tensor", out=Xn, in0=st["Xp"], in1=st["XT"], op=ALU.add)
            st["XT"] = Xn; st["lvl"] += 1

        def s9():
            pu = psq(); pw = psq()
            P.op("tensor", "matmul", out=pu, lhsT=st["XT"], rhs=w["vb"], start=True, stop=True)
            P.op("tensor", "matmul", out=pw, lhsT=w["kbg"], rhs=st["XT"], start=True, stop=True)
            P.op("scalar", "copy", out=u_all[:, n, :], in_=pu)
            P.op("vector", "tensor_copy", out=wT_all[:, tok], in_=pw)
        steps = [s1, s2, s3, s4]
        for _ in range(6):
            steps += [sq_a, sq_b, sq_c]
        steps.append(s9)
        return steps

    for h in range(4):
        for c3 in range(3):
            P.dma("sync", out=rawb[:, c3, 4:2052], in_=raw[h*3 + c3])
        for c3 in range(3):
            ch = h*3 + c3
            eng = "vector"
            P.op(eng, "tensor_scalar", out=tmpc[:, 0:2048], in0=rawb[:, c3, 1:2049], scalar1=cw_sb[:, ch*4:ch*4+1],
                 scalar2=None, op0=ALU.mult)
            for j in range(1, 4):
                P.op(eng, "scalar_tensor_tensor", out=tmpc[:, 0:2048], in0=rawb[:, c3, 1+j:2049+j],
                     scalar=cw_sb[:, ch*4+j:ch*4+j+1], in1=tmpc[:, 0:2048], op0=ALU.mult, op1=ALU.add)
            P.op("scalar", "activation", out=QKV[:, c3, :], in_=tmpc[:, 0:2048], func=AF.Silu)
            if c3 < 2:
                for t4 in range(4):
                    ts = slice(t4*512, (t4+1)*512)
                    P.op("scalar", "activation", out=sq, in_=QKV[:, c3, ts], func=AF.Square)
                    pp = psbank()
                    P.op("tensor", "matmul", out=pp[:, :], lhsT=ones, rhs=sq, start=True, stop=True)
                    P.op("scalar", "activation", out=rs, in_=pp[:, :], func=AF.Ln, bias=cb[:, 0:1])
                    P.op("scalar", "activation", out=rs, in_=rs, func=AF.Exp, scale=-0.5)
                    P.op("vector", "scalar_tensor_tensor", out=QKV[:, c3, ts], in0=rs, scalar=(128 ** -0.5 if c3 == 0 else 1.0),
                         in1=QKV[:, c3, ts], op0=ALU.mult, op1=ALU.mult)
        if stop == "G0":
            return P.finish()
        for n in range(NT):
            P.op("gpsimd", "tensor_tensor", out=nz[:, n, :], in0=sz[:, n*512 + h*128:n*512 + (h+1)*128], in1=nw_sb[:], op=ALU.mult)
        for n0 in range(0, NT, NSL):
            lists = [phase1_steps(h, n0 + s, s) for s in range(NSL)]
            for i in range(len(lists[0])):
                if stop is not None and stop.startswith("S") and i >= int(stop[1:]):
                    return P.finish()
                for s in range(NSL):
                    lists[s][i]()
        if stop == "G1":
            return P.finish()
        P.op("vector", "memset", ap=Sst[0], constant=0.0)
        for n in range(NT):
            tok = slice(n*128, (n+1)*128)
            S = Sst[n % 2]; Sn = Sst[(n + 1) % 2]
            pws = psq()
            P.op("tensor", "matmul", out=pws, lhsT=wT_all[:, tok], rhs=S, start=True, stop=True)
            vnew = W[0]["t3"] if n % 2 == 0 else W[1]["t3"]
            P.op("vector", "tensor_tensor", out=vnew, in0=u_all[:, n, :], in1=pws, op=ALU.subtract)
            po = psq(); pd = psq()
            P.op("tensor", "matmul", out=po, lhsT=QgT_all[:, tok], rhs=S, start=True, stop=False)
            P.op("tensor", "matmul", out=po, lhsT=qkT_all[:, n, :], rhs=vnew, start=False, stop=True)
            P.op("tensor", "matmul", out=pd, lhsT=kdec_all[:, n, :], rhs=vnew, start=True, stop=True)
            P.op("vector", "scalar_tensor_tensor", out=Sn, in0=S, scalar=egl_all[:, n:n+1], in1=pd, op0=ALU.mult, op1=ALU.add)
            ssq = W[n % 2]["cols"][:, 8:9]; rstd = W[n % 2]["cols"][:, 9:10]
            junk = W[n % 2]["t4"]
            P.op("scalar", "activation", out=junk, in_=po, func=AF.Square, accum_out=ssq)
            P.op("scalar", "activation", out=rstd, in_=ssq, func=AF.Ln, scale=1.0 / 128, bias=cb[:, 0:1])
            P.op("scalar", "activation", out=rstd, in_=rstd, func=AF.Exp, scale=-0.5)
            P.op("vector", "scalar_tensor_tensor", out=ya_h[:, n, :], in0=po, scalar=rstd, in1=nz[:, n, :], op0=ALU.mult, op1=ALU.mult)
        P.dma("sync", out=ya[:, h*128:(h+1)*128].rearrange("(n p) e -> p n e", p=128), in_=ya_h, slot="yast")

        if stop == "G2":
            return P.finish()
    bg = rawb
    for c in range(4):
        for c3 in range(3):
            P.dma("sync", out=rawb[:, c3, 4:2052], in_=raw[12 + c*3 + c3])
        P.op("vector", "memset", ap=tmpc[:, 0:4], constant=0.0)
        P.op("vector", "tensor_tensor", out=tmpc[:, 4:2052], in0=rawb[:, 1, 4:2052], in1=rawb[:, 2, 4:2052], op=ALU.mult)
        acc = QKV[:, 0, :]
        P.op("vector", "tensor_scalar", out=acc, in0=tmpc[:, 2:2050], scalar1=scw_sb[:, c*3:c*3+1], scalar2=None, op0=ALU.mult)
        for j in range(1, 3):
            P.op("vector", "scalar_tensor_tensor", out=acc, in0=tmpc[:, 2+j:2050+j], scalar=scw_sb[:, c*3+j:c*3+j+1],
                 in1=acc, op0=ALU.mult, op1=ALU.add)
        ob = QKV[:, 1 + c % 2, :]
        P.op("gpsimd", "tensor_tensor", out=ob, in0=acc, in1=rawb[:, 0, 4:2052], op=ALU.mult)
        P.dma("sync", out=ybT[c*128:(c+1)*128, :], in_=ob, slot="ybst%d" % (c % 2))
    return P.finish()


def host_inputs_B(inp, mod):
    w_in = inp["hyb_w_in"][0]
    cst = consts_np()
    maps = []
    G = 1024
    for core in range(8):
        b, hh = core // 2, core % 2
        xT = np.ascontiguousarray(inp["x"][b].T.reshape(16, 128, 2048).transpose(1, 0, 2))
        sh1 = mod[0, b, 0:2048]; sc1 = mod[0, b, 2048:4096]
        modc = np.ascontiguousarray(np.concatenate([sc1.reshape(16, 128).T, sh1.reshape(16, 128).T], axis=1))
        groups = []
        for hl in range(4):
            h = hh*4 + hl
            cols = np.concatenate([np.arange(h*128, (h+1)*128) + o for o in (0, G, 2*G)])
            groups.append(w_in[:, cols])
        base = 4*G + 16
        for cl in range(4):
            c0 = hh*512 + cl*128
            cols = np.concatenate([np.arange(c0, c0+128) + base + o for o in (0, G, 2*G)])
            groups.append(w_in[:, cols])
        wf = np.ascontiguousarray(np.stack(groups))
        wz = np.ascontiguousarray(w_in[:, 3*G + hh*512:3*G + (hh+1)*512])
        bcols = np.concatenate([4*G + hh*4 + np.arange(4), 4*G + 8 + hh*4 + np.arange(4)])
        wba = np.ascontiguousarray(w_in[:, bcols].reshape(16, 128, 8).transpose(1, 0, 2))
        cwf = inp["gdn_conv_w"][0]
        cw = np.zeros((128, 12, 4), np.float32)
        for hl in range(4):
            h = hh*4 + hl
            for c3 in range(3):
                cw[:, hl*3 + c3, :] = cwf[:, c3*G + h*128:c3*G + (h+1)*128].T
        scf = inp["sc_conv_w"][0]
        scw = np.zeros((128, 4, 3), np.float32)
        for cl in range(4):
            c0 = hh*512 + cl*128
            scw[:, cl, :] = scf[:, c0:c0+128].T
        hv = np.tile(np.concatenate([inp["gdn_a_log"][0][hh*4:hh*4+4], inp["gdn_dt_bias"][0][hh*4:hh*4+4]])[None, :], (128, 1)).astype(np.float32)
        nw = np.tile(inp["gdn_norm_w"][0][None, :], (128, 1)).astype(np.float32)
        maps.append({"xT": xT, "modc": modc, "wf": wf, "wz": wz, "wba": wba, "cw": cw, "scw": scw,
                     "hv": np.ascontiguousarray(hv), "nw": np.ascontiguousarray(nw), "cst": cst})
    return maps


def gather_B(results):
    ycat = np.zeros((4, 2048, 2048), np.float32)
    for core in range(8):
        b, hh = core // 2, core % 2
        ycat[b, :, hh*512:(hh+1)*512] = results[core]["ya"]
        ycat[b, :, 1024 + hh*512:1024 + (hh+1)*512] = results[core]["ybT"].T
    return ycat


import numpy as np

DN_ALPHA = 4.0 ** 0.25
D = 2048


def build_post(n_experts=64):
    nc = bass.Bass("TRN2", target_bir_lowering=False)
    P = Prog(nc)
    ycT = nc.dram_tensor("ycT", [128, 16, 1024], F32, kind="ExternalInput").ap()
    xin = nc.dram_tensor("xin", [1024, 2048], F32, kind="ExternalInput").ap()
    wout = nc.dram_tensor("wout", [2048, 2048], F32, kind="ExternalInput").ap()
    bc1 = nc.dram_tensor("bc1", [128, 3, 2048], F32, kind="ExternalInput").ap()
    bc2 = nc.dram_tensor("bc2", [128, 3, 2048], F32, kind="ExternalInput").ap()
    colv = nc.dram_tensor("colv", [128, 32], F32, kind="ExternalInput").ap()
    wr = nc.dram_tensor("wr", [128, 16, 72], F32, kind="ExternalInput").ap()
    br = nc.dram_tensor("br", [128, 72], F32, kind="ExternalInput").ap()
    wg = nc.dram_tensor("wg", [n_experts, 2048, 512], F32, kind="ExternalInput").ap()
    wu = nc.dram_tensor("wu", [n_experts, 2048, 512], F32, kind="ExternalInput").ap()
    wd = nc.dram_tensor("wd", [n_experts, 512, 2048], F32, kind="ExternalInput").ap()
    idn = nc.dram_tensor("idn", [128, 128], F32, kind="ExternalInput").ap()
    xo = nc.dram_tensor("xo", [1024, 2048], F32, kind="ExternalOutput").ap()
    x1s = P.dram("x1scr", [1024, 2048], F32)

    ident = P.sb("ident", [128, 128])
    cb = P.sb("cb", [128, 4])
    colv_sb = P.sb("colv_sb", [128, 32]); sc2p = P.sb("sc2p", [128, 16])
    wr_sb = P.sb("wr_sb", [128, 16 * 72]); br_sb = P.sb("br_sb", [128, 72])
    gates = P.sb("gates", [128, 8 * 64])
    sm = P.sb("sm", [128, 512])
    A = P.sb("arena", [128, 43008])
    psb = [P.ps("ps%d" % i, [128, 512]) for i in range(8)]

    yacc = A[:, 0:16384].rearrange("p (n d) -> p n d", n=8)
    woutb = A[:, 0:16384].bitcast(BF16).rearrange("p (k n) -> p k n", k=16)
    h2T = A[:, 16384:24576].bitcast(BF16).rearrange("p (k t) -> p k t", k=16)
    W0 = 24576
    BC = A[:, 36864:43008].rearrange("p (c d) -> p c d", c=3)
    ycb = [A[:, W0 + i*1024:W0 + (i+1)*1024].bitcast(BF16).rearrange("p (k t) -> p k t", k=16) for i in range(2)]
    xt = A[:, W0 + 2048:W0 + 4096]
    r = A[:, W0 + 4096:W0 + 6144]
    h2f = A[:, W0 + 6144:W0 + 8192].rearrange("p (k t) -> p k t", k=16)
    junk = A[:, W0 + 8192:W0 + 10240]
    wgb = [A[:, W0 + i*6144:W0 + i*6144 + 2048].bitcast(BF16).rearrange("p (k n) -> p k n", k=16) for i in range(2)]
    wub = [A[:, W0 + i*6144 + 2048:W0 + i*6144 + 4096].bitcast(BF16).rearrange("p (k n) -> p k n", k=16) for i in range(2)]
    wdb = [A[:, W0 + i*6144 + 4096:W0 + i*6144 + 6144].bitcast(BF16).rearrange("p (c n) -> p c n", c=2) for i in range(2)]
    hidT = A[:, 36864:36864 + 2048].bitcast(BF16).rearrange("p (c t) -> p c t", c=4)
    sgb = [A[:, 36864 + 2048 + i*256:36864 + 2048 + (i+1)*256].bitcast(BF16) for i in range(2)]

    P.dma("sync", out=ident[:], in_=idn)
    P.dma("sync", out=colv_sb[:], in_=colv)
    P.dma("sync", out=wr_sb[:], in_=wr.rearrange("p k n -> p (k n)"))
    P.dma("sync", out=br_sb[:], in_=br)
    P.dma("sync", out=BC, in_=bc1)
    P.op("vector", "memset", ap=cb[:, 0:1], constant=1e-5)
    P.op("vector", "tensor_scalar", out=sc2p[:], in0=colv_sb[:, 0:16], scalar1=1.0, scalar2=None, op0=ALU.add)
    P.op("vector", "tensor_scalar", out=BC[:, 0, :], in0=BC[:, 0, :], scalar1=1.0, scalar2=None, op0=ALU.add)
    for kg in range(4):
        P.dma("gpsimd", out=woutb[:, kg*4:(kg+1)*4, :], in_=wout[kg*512:(kg+1)*512, :].rearrange("(k p) n -> p k n", p=128),
              slot="wout")

    def ln_tile(src_r, n, gcol, bcol, dst):
        c0 = (n % 2) * 16
        s1 = sm[:, c0:c0+1]; s2 = sm[:, c0+1:c0+2]; nm = sm[:, c0+2:c0+3]; msq = sm[:, c0+3:c0+4]
        var = sm[:, c0+4:c0+5]; rstd = sm[:, c0+5:c0+6]; nb = sm[:, c0+6:c0+7]
        P.op("scalar", "activation", out=junk, in_=src_r, func=AF.Identity, accum_out=s1)
        P.op("scalar", "activation", out=junk, in_=src_r, func=AF.Square, accum_out=s2)
        P.op("vector", "tensor_scalar", out=nm, in0=s1, scalar1=-1.0 / D, scalar2=None, op0=ALU.mult)
        P.op("vector", "tensor_tensor", out=msq, in0=nm, in1=nm, op=ALU.mult)
        P.op("vector", "tensor_scalar", out=var, in0=s2, scalar1=1.0 / D, scalar2=msq, op0=ALU.mult, op1=ALU.subtract)
        P.op("scalar", "activation", out=rstd, in_=var, func=AF.Ln, bias=cb[:, 0:1])
        P.op("scalar", "activation", out=rstd, in_=rstd, func=AF.Exp, scale=-0.5)
        P.op("vector", "tensor_tensor", out=nb, in0=nm, in1=rstd, op=ALU.mult)
        P.op("scalar", "activation", out=dst, in_=src_r, func=AF.Identity, scale=rstd, bias=nb)
        P.op("vector", "tensor_tensor", out=dst, in0=dst, in1=BC[:, gcol, :], op=ALU.mult)
        P.op("gpsimd", "tensor_tensor", out=dst, in0=dst, in1=BC[:, bcol, :], op=ALU.add)

    for n in range(8):
        tok = slice(n*128, (n+1)*128)
        yb = ycb[n % 2]
        P.dma("gpsimd", out=yb, in_=ycT[:, :, tok], slot="ycb%d" % (n % 2))
        P.dma("sync", out=xt, in_=xin[tok, :])
        for cg in range(4):
            for k in range(16):
                P.op("tensor", "matmul", out=psb[cg][:, :], lhsT=yb[:, k, :], rhs=woutb[:, k, cg*512:(cg+1)*512],
                     start=(k == 0), stop=(k == 15))
        for cg in range(4):
            cs = slice(cg*512, (cg+1)*512)
            P.op("vector", "tensor_tensor", out=r[:, cs], in0=psb[cg][:, :], in1=BC[:, 0, cs], op=ALU.mult)
        P.op("vector", "scalar_tensor_tensor", out=r, in0=xt, scalar=DN_ALPHA, in1=r, op0=ALU.mult, op1=ALU.add)
        ln_tile(r, n, 1, 2, r)
        P.dma("sync", out=x1s[tok, :], in_=r, slot="x1st")
        for k in range(16):
            pt = psb[4 + (k % 2)][:, (k // 2 % 4)*128:(k // 2 % 4 + 1)*128]
            P.op("tensor", "transpose", out=pt, in_=r[:, k*128:(k+1)*128], identity=ident[:])
            P.op("scalar", "activation", out=h2f[:, k, :], in_=pt, func=AF.Identity, scale=sc2p[:, k:k+1],
                 bias=colv_sb[:, 16 + k:17 + k])
            P.op("vector" if k % 2 else "gpsimd", "tensor_copy", out=h2T[:, k, tok], in_=h2f[:, k, :])
        pr = psb[6][:, 0:72]
        for k in range(16):
            P.op("tensor", "matmul", out=pr, lhsT=h2f[:, k, :], rhs=wr_sb[:, k*72:(k+1)*72], start=(k == 0), stop=(k == 15))
        o = 256 + (n % 2) * 128
        lg = sm[:, o:o+72]; tmp3 = sm[:, 192:256].rearrange("p (g j) -> p g j", g=8)
        c1 = o + 112
        gmax = sm[:, c1:c1+1]; ngmax = sm[:, c1+1:c1+2]; sumg = sm[:, c1+2:c1+3]; nm1 = sm[:, c1+3:c1+4]
        den = sm[:, c1+4:c1+5]; rec = sm[:, c1+5:c1+6]
        ohg = sm[:, o+72:o+80]; lsel = sm[:, o+80:o+88]; top8 = sm[:, o+88:o+96]; mask2 = sm[:, o+96:o+104]
        ee = sm[:, o+104:o+112]
        P.op("vector", "tensor_tensor", out=lg, in0=pr, in1=br_sb[:], op=ALU.add)
        P.op("vector", "tensor_reduce", out=gmax, in_=lg[:, 0:8], axis=AX.X, op=ALU.max)
        P.op("vector", "tensor_scalar", out=ohg, in0=lg[:, 0:8], scalar1=gmax, scalar2=None, op0=ALU.is_ge)
        P.op("vector", "tensor_scalar", out=ngmax, in0=gmax, scalar1=-1.0, scalar2=None, op0=ALU.mult)
        P.op("scalar", "activation", out=top8, in_=lg[:, 0:8], func=AF.Exp, bias=ngmax, accum_out=sumg)
        le3 = lg[:, 8:72].rearrange("p (g j) -> p g j", g=8)
        P.op("vector", "tensor_tensor", out=tmp3, in0=le3, in1=ohg.unsqueeze(2).to_broadcast([128, 8, 8]), op=ALU.mult)
        P.op("vector", "tensor_reduce", out=lsel, in_=sm[:, 192:256].rearrange("p (g j) -> p j g", g=8), axis=AX.X, op=ALU.add)
        P.op("vector", "max", out=top8, in_=lsel)
        P.op("vector", "tensor_scalar", out=mask2, in0=lsel, scalar1=top8[:, 1:2], scalar2=None, op0=ALU.is_ge)
        P.op("vector", "tensor_scalar", out=nm1, in0=top8[:, 0:1], scalar1=-1.0, scalar2=None, op0=ALU.mult)
        P.op("scalar", "activation", out=ee, in_=lsel, func=AF.Exp, bias=nm1)
        P.op("vector", "tensor_tensor", out=ee, in0=ee, in1=mask2, op=ALU.mult)
        P.op("vector", "tensor_reduce", out=den, in_=ee, axis=AX.X, op=ALU.add)
        P.op("vector", "tensor_tensor", out=den, in0=den, in1=sumg, op=ALU.mult)
        P.op("vector", "reciprocal", out=rec, in_=den)
        P.op("vector", "tensor_scalar", out=ee, in0=ee, scalar1=rec, scalar2=None, op0=ALU.mult)
        g3 = gates[:, n*64:(n+1)*64].rearrange("p (g j) -> p g j", g=8)
        P.op("vector", "tensor_tensor", out=g3, in0=ohg.unsqueeze(2).to_broadcast([128, 8, 8]),
             in1=ee.unsqueeze(1).to_broadcast([128, 8, 8]), op=ALU.mult)

    for e in range(n_experts):
        for hf in range(2):
            P.dma("gpsimd", out=wgb[hf], in_=wg[e, :, hf*256:(hf+1)*256].rearrange("(k p) n -> p k n", p=128), slot="wg%d" % hf)
            P.dma("gpsimd", out=wub[hf], in_=wu[e, :, hf*256:(hf+1)*256].rearrange("(k p) n -> p k n", p=128), slot="wu%d" % hf)
            P.dma("gpsimd", out=wdb[hf], in_=wd[e, hf*256:(hf+1)*256, :].rearrange("(c p) n -> p c n", p=128), slot="wd%d" % hf)
        it = 0
        for hf in range(2):
            for fc in range(2):
                c = hf*2 + fc
                for tg in range(2):
                    pg = psb[(it % 2) * 2]; pu = psb[(it % 2) * 2 + 1]; it += 1
                    ts = slice(tg*512, (tg+1)*512)
                    for k in range(16):
                        P.op("tensor", "matmul", out=pg[:, :], lhsT=wgb[hf][:, k, fc*128:(fc+1)*128], rhs=h2T[:, k, ts],
                             start=(k == 0), stop=(k == 15))
                    for k in range(16):
                        P.op("tensor", "matmul", out=pu[:, :], lhsT=wub[hf][:, k, fc*128:(fc+1)*128], rhs=h2T[:, k, ts],
                             start=(k == 0), stop=(k == 15))
                    sg = sgb[tg]
                    P.op("scalar", "activation", out=sg, in_=pg[:, :], func=AF.Silu)
                    P.op("vector", "tensor_tensor", out=hidT[:, c, ts], in0=sg, in1=pu[:, :], op=ALU.mult)
        it = 0
        for n in range(8):
            for hc in range(2):
                pd = [psb[4 + (it % 2) * 2], psb[5 + (it % 2) * 2]]; it += 1
                for j in range(2):
                    for c in range(4):
                        P.op("tensor", "matmul", out=pd[j][:, :], lhsT=hidT[:, c, n*128:(n+1)*128],
                             rhs=wdb[c // 2][:, c % 2, hc*1024 + j*512:hc*1024 + (j+1)*512], start=(c == 0), stop=(c == 3))
                gcol = gates[:, n*64 + e:n*64 + e + 1]
                for j in range(2):
                    ys = yacc[:, n, hc*1024 + j*512:hc*1024 + (j+1)*512]
                    if e == 0:
                        P.op("vector", "tensor_scalar", out=ys, in0=pd[j][:, :], scalar1=gcol, scalar2=None, op0=ALU.mult)
                    else:
                        P.op("vector", "scalar_tensor_tensor", out=ys, in0=pd[j][:, :], scalar=gcol, in1=ys,
                             op0=ALU.mult, op1=ALU.add)

    P.dma("sync", out=BC, in_=bc2)
    P.op("vector", "tensor_scalar", out=BC[:, 0, :], in0=BC[:, 0, :], scalar1=1.0, scalar2=None, op0=ALU.add)
    for n in range(8):
        tok = slice(n*128, (n+1)*128)
        P.dma("sync", out=xt, in_=x1s[tok, :])
        P.op("vector", "tensor_tensor", out=r, in0=yacc[:, n, :], in1=BC[:, 0, :], op=ALU.mult)
        P.op("vector", "scalar_tensor_tensor", out=r, in0=xt, scalar=DN_ALPHA, in1=r, op0=ALU.mult, op1=ALU.add)
        ln_tile(r, n, 1, 2, r)
        P.dma("sync", out=xo[tok, :], in_=r, slot="xost")
    return P.finish()


def host_inputs_post(inp, mod, layer, ycat, xres):
    wout = inp["hyb_w_out"][0] if layer == 0 else inp["mla_w_out"][0]
    idn = np.eye(128, dtype=np.float32)
    wr = np.concatenate([inp["moe_router_g"][layer], inp["moe_router_e"][layer]], axis=1)
    wr = np.ascontiguousarray(wr.reshape(16, 128, 72).transpose(1, 0, 2))
    brv = np.concatenate([inp["moe_bias_g"][layer], inp["moe_bias_e"][layer]])
    br = np.ascontiguousarray(np.tile(brv[None, :], (128, 1)).astype(np.float32))
    maps = []
    for core in range(8):
        b, half = core // 2, core % 2
        ts = slice(half*1024, (half+1)*1024)
        m = mod[layer, b]
        sh1, sc1, g1, sh2, sc2, g2 = (m[i*2048:(i+1)*2048] for i in range(6))
        ycT = np.ascontiguousarray(ycat[b, ts, :].T.reshape(16, 128, 1024).transpose(1, 0, 2))
        bc1 = np.ascontiguousarray(np.tile(np.stack([g1, inp["ln_g"][layer, 0], inp["ln_b"][layer, 0]])[None], (128, 1, 1)).astype(np.float32))
        bc2 = np.ascontiguousarray(np.tile(np.stack([g2, inp["ln_g"][layer, 1], inp["ln_b"][layer, 1]])[None], (128, 1, 1)).astype(np.float32))
        colv = np.ascontiguousarray(np.concatenate([sc2.reshape(16, 128).T, sh2.reshape(16, 128).T], axis=1))
        maps.append({"ycT": ycT, "xin": np.ascontiguousarray(xres[b, ts, :]), "wout": wout, "bc1": bc1, "bc2": bc2,
                     "colv": colv, "wr": wr, "br": br, "wg": inp["moe_w_gate"][layer], "wu": inp["moe_w_up"][layer],
                     "wd": inp["moe_w_down"][layer], "idn": idn})
    return maps


def gather_post(results):
    x = np.zeros((4, 2048, 2048), np.float32)
    for core in range(8):
        b, half = core // 2, core % 2
        x[b, half*1024:(half+1)*1024, :] = results[core]["xo"]
    return x


import numpy as np, math

SCALE = 192.0 ** -0.5
C1 = 6.28125
C2 = 2.0 * math.pi - 6.28125


def build_F():
    nc = bass.Bass("TRN2", target_bir_lowering=False)
    P = Prog(nc)
    xT = nc.dram_tensor("xT", [128, 16, 2048], F32, kind="ExternalInput").ap()
    modc = nc.dram_tensor("modc", [128, 32], F32, kind="ExternalInput").ap()
    win = nc.dram_tensor("win", [2048, 1088], F32, kind="ExternalInput").ap()
    nbc = nc.dram_tensor("nbc", [128, 1024], F32, kind="ExternalInput").ap()
    wuq = nc.dram_tensor("wuq", [512, 1536], F32, kind="ExternalInput").ap()
    wuk = nc.dram_tensor("wuk", [512, 1024], F32, kind="ExternalInput").ap()
    wuv = nc.dram_tensor("wuv", [512, 1024], F32, kind="ExternalInput").ap()
    posb = nc.dram_tensor("posb", [32, 2048], I32, kind="ExternalInput").ap()
    invc = nc.dram_tensor("invc", [32, 1], F32, kind="ExternalInput").ap()
    cst = nc.dram_tensor("cst", [128, 256], F32, kind="ExternalInput").ap()
    yc = nc.dram_tensor("yc", [2048, 1024], F32, kind="ExternalOutput").ap()

    csb = P.sb("csb", [128, 256]); ident = csb[:, 0:128]; cmask = csb[:, 128:256]
    identb = P.sb("identb", [128, 128], BF16)
    cb = P.sb("cb", [128, 4])
    mod_sb = P.sb("mod_sb", [128, 32]); scp1 = P.sb("scp1", [128, 16])
    nbc_sb = P.sb("nbc_sb", [128, 1024])
    sm = P.sb("sm", [128, 64])
    inv_sb = P.sb("inv_sb", [32, 1])
    cosT = P.sb("cosT", [32, 2048]); sinT = P.sb("sinT", [32, 2048])
    ang = P.sb("ang", [32, 512]); tf = P.sb("tf", [32, 512]); ti = P.sb("ti", [32, 512], I32); ta = P.sb("ta", [32, 512])
    kr = [P.sb("kr%d" % i, [32, 2048], BF16) for i in range(2)]
    kp = [P.sb("kp%d" % i, [32, 512]) for i in range(2)]
    cqnT = P.sb("cqnT", [128, 4 * 2048], BF16); ckvnT = P.sb("ckvnT", [128, 4 * 2048], BF16)
    cq3 = cqnT[:, :].rearrange("p (k t) -> p k t", k=4); ckv3 = ckvnT[:, :].rearrange("p (k t) -> p k t", k=4)
    wuqb = [P.sb("wuqb%d" % i, [128, 4 * 192], BF16) for i in range(2)]
    wukb = [P.sb("wukb%d" % i, [128, 4 * 128], BF16) for i in range(2)]
    wuvb = [P.sb("wuvb%d" % i, [128, 4 * 128], BF16) for i in range(2)]
    A = P.sb("arena", [128, 28160])
    posi = A[0:32, 16384:18432].bitcast(I32)
    psb = [P.ps("ps%d" % i, [128, 512]) for i in range(6)]
    ptb = P.ps("ptb", [128, 1024], BF16)
    pvo = P.ps("pvo", [128, 512])
    pbi = [0]

    def bank():
        i = pbi[0]; pbi[0] = (i + 1) % 6
        return psb[i]

    P.dma("sync", out=csb[:], in_=cst)
    P.dma("sync", out=mod_sb[:], in_=modc)
    P.dma("sync", out=nbc_sb[:], in_=nbc)
    P.dma("sync", out=inv_sb[:], in_=invc)
    P.dma("sync", out=posi, in_=posb)
    P.op("vector", "memset", ap=cb[:, 0:1], constant=1e-6)
    P.op("vector", "tensor_copy", out=identb[:], in_=ident)
    P.op("vector", "tensor_scalar", out=scp1[:], in0=mod_sb[:, 0:16], scalar1=1.0, scalar2=None, op0=ALU.add)

    def sin_of(dst, shift, ts):
        a = ta
        P.op("vector", "tensor_copy", out=tf[:], in_=posi[:, ts])
        P.op("vector", "tensor_scalar", out=a[:], in0=tf[:], scalar1=inv_sb[:, 0:1], scalar2=shift, op0=ALU.mult, op1=ALU.add)
        P.op("vector", "tensor_scalar", out=tf[:], in0=a[:], scalar1=1.0 / (2 * math.pi), scalar2=None, op0=ALU.mult)
        P.op("vector", "tensor_copy", out=ti[:], in_=tf[:])
        P.op("vector", "tensor_copy", out=tf[:], in_=ti[:])
        P.op("vector", "scalar_tensor_tensor", out=a[:], in0=tf[:], scalar=-C1, in1=a[:], op0=ALU.mult, op1=ALU.add)
        P.op("vector", "scalar_tensor_tensor", out=a[:], in0=tf[:], scalar=-C2, in1=a[:], op0=ALU.mult, op1=ALU.add)
        P.op("vector", "tensor_scalar", out=tf[:], in0=a[:], scalar1=math.pi, scalar2=-2 * math.pi, op0=ALU.is_gt, op1=ALU.mult)
        P.op("vector", "tensor_tensor", out=a[:], in0=a[:], in1=tf[:], op=ALU.add)
        P.op("vector", "tensor_scalar", out=tf[:], in0=a[:], scalar1=-math.pi, scalar2=2 * math.pi, op0=ALU.is_lt, op1=ALU.mult)
        P.op("vector", "tensor_tensor", out=a[:], in0=a[:], in1=tf[:], op=ALU.add)
        P.op("scalar", "activation", out=dst[:, ts], in_=a[:], func=AF.Sin)
    for t4 in range(4):
        sin_of(sinT, 0.0, slice(t4*512, (t4+1)*512))
        sin_of(cosT, math.pi / 2, slice(t4*512, (t4+1)*512))

    def do_rope(x1, x2, o1, o2, ts):
        P.op("vector", "tensor_tensor", out=tf[:], in0=x1, in1=cosT[:, ts], op=ALU.mult)
        P.op("gpsimd", "tensor_tensor", out=ang[:], in0=x2, in1=sinT[:, ts], op=ALU.mult)
        P.op("vector", "tensor_tensor", out=o1, in0=tf[:], in1=ang[:], op=ALU.subtract)
        P.op("vector", "tensor_tensor", out=tf[:], in0=x1, in1=sinT[:, ts], op=ALU.mult)
        P.op("gpsimd", "tensor_tensor", out=ang[:], in0=x2, in1=cosT[:, ts], op=ALU.mult)
        P.op("vector", "tensor_tensor", out=o2, in0=tf[:], in1=ang[:], op=ALU.add)

    hinT = A[:, 0:16384].bitcast(BF16).rearrange("p (k t) -> p k t", k=16)
    xt = A[:, 16384:18432].rearrange("p (k t) -> p k t", k=16)
    winb = A[:, 18432:18432 + 8704].bitcast(BF16).rearrange("p (k n) -> p k n", k=16)
    P.dma("gpsimd", out=winb, in_=win.rearrange("(k p) n -> p k n", p=128))
    for tt in range(16):
        t0 = tt * 128
        P.dma("sync", out=xt, in_=xT[:, :, t0:t0 + 128])
        for k in range(16):
            P.op("scalar", "activation", out=hinT[:, k, t0:t0 + 128], in_=xt[:, k, :], func=AF.Identity,
                 scale=scp1[:, k:k+1], bias=mod_sb[:, 16 + k:17 + k])
    cn = A[:, 18432 + 8704:18432 + 8704 + 1024]
    for n in range(16):
        tok = slice(n*128, (n+1)*128)
        pc = [bank(), bank()]
        for j in range(2):
            for k in range(16):
                P.op("tensor", "matmul", out=pc[j][:, :], lhsT=hinT[:, k, tok], rhs=winb[:, k, j*512:(j+1)*512],
                     start=(k == 0), stop=(k == 15))
        for j in range(2):
            ss = sm[:, j*4:j*4+1]; rstd = sm[:, j*4+1:j*4+2]
            P.op("scalar", "activation", out=cn[:, j*512:(j+1)*512], in_=pc[j][:, :], func=AF.Square, accum_out=ss)
            P.op("scalar", "activation", out=rstd, in_=ss, func=AF.Ln, scale=1.0 / 512, bias=cb[:, 0:1])
            P.op("scalar", "activation", out=rstd, in_=rstd, func=AF.Exp, scale=-0.5)
            P.op("scalar", "activation", out=cn[:, j*512:(j+1)*512], in_=pc[j][:, :], func=AF.Identity, scale=rstd)
            P.op("vector", "tensor_tensor", out=cn[:, j*512:(j+1)*512], in0=cn[:, j*512:(j+1)*512],
                 in1=nbc_sb[:, j*512:(j+1)*512], op=ALU.mult)
        for j in range(2):
            pt = bank()
            for c in range(4):
                P.op("tensor", "transpose", out=pt[:, c*128:(c+1)*128], in_=cn[:, j*512 + c*128:j*512 + (c+1)*128], identity=ident)
            dst = cq3 if j == 0 else ckv3
            for c in range(4):
                P.op("vector" if c % 2 else "scalar", "tensor_copy" if c % 2 else "copy", out=dst[:, c, tok], in_=pt[:, c*128:(c+1)*128])
    for t4 in range(4):
        ts = slice(t4*512, (t4+1)*512)
        for hf in range(2):
            pk = bank()
            for k in range(16):
                P.op("tensor", "matmul", out=pk[0:32, :], lhsT=winb[:, k, 1024 + hf*32:1024 + (hf+1)*32], rhs=hinT[:, k, ts],
                     start=(k == 0), stop=(k == 15))
            P.op("vector", "tensor_copy", out=kp[hf][:], in_=pk[0:32, :])
        do_rope(kp[0][:], kp[1][:], kr[0][:, ts], kr[1][:, ts], ts)

    o0 = 0
    qTn = A[:, o0:o0 + 1024].bitcast(BF16); o0 += 1024
    kTn = A[:, o0:o0 + 1024].bitcast(BF16); o0 += 1024
    Vh = A[:, o0:o0 + 1024].bitcast(BF16).rearrange("p (n e) -> p n e", n=16); o0 += 1024
    q12 = [A[0:32, o0 + i*2048:o0 + (i+1)*2048] for i in range(2)]; o0 += 4096
    qr = [A[0:32, o0 + i*1024:o0 + (i+1)*1024].bitcast(BF16) for i in range(2)]; o0 += 2048
    Ssb = [A[:, o0 + i*2048:o0 + (i+1)*2048] for i in range(2)]; o0 += 4096
    Pb = [A[:, o0 + i*1024:o0 + (i+1)*1024].bitcast(BF16) for i in range(2)]; o0 += 2048
    PTs = [A[:, o0 + i*64:o0 + (i+1)*64].bitcast(BF16) for i in range(4)]; o0 += 256
    osb = [A[:, o0 + i*128:o0 + (i+1)*128] for i in range(2)]; o0 += 256
    for h in range(8):
        wq3 = wuqb[h % 2][:, :].rearrange("p (k n) -> p k n", k=4)
        wk3 = wukb[h % 2][:, :].rearrange("p (k n) -> p k n", k=4)
        wv3 = wuvb[h % 2][:, :].rearrange("p (k n) -> p k n", k=4)
        P.dma("gpsimd", out=wq3, in_=wuq[:, h*192:(h+1)*192].rearrange("(k p) n -> p k n", p=128))
        P.dma("gpsimd", out=wk3, in_=wuk[:, h*128:(h+1)*128].rearrange("(k p) n -> p k n", p=128))
        P.dma("gpsimd", out=wv3, in_=wuv[:, h*128:(h+1)*128].rearrange("(k p) n -> p k n", p=128))
        for t4 in range(4):
            ts = slice(t4*512, (t4+1)*512)
            pq = bank()
            for kc in range(4):
                P.op("tensor", "matmul", out=pq[:, :], lhsT=wq3[:, kc, 0:128], rhs=cq3[:, kc, ts], start=(kc == 0), stop=(kc == 3))
            P.op("scalar", "copy", out=qTn[:, ts], in_=pq[:, :])
            for hf in range(2):
                p2 = bank()
                for kc in range(4):
                    P.op("tensor", "matmul", out=p2[0:32, :], lhsT=wq3[:, kc, 128 + hf*32:160 + hf*32], rhs=cq3[:, kc, ts],
                         start=(kc == 0), stop=(kc == 3))
                P.op("vector", "tensor_copy", out=q12[hf][:, ts], in_=p2[0:32, :])
            pk = bank()
            for kc in range(4):
                P.op("tensor", "matmul", out=pk[:, :], lhsT=wk3[:, kc, :], rhs=ckv3[:, kc, ts], start=(kc == 0), stop=(kc == 3))
            P.op("scalar", "copy", out=kTn[:, ts], in_=pk[:, :])
        for t4 in range(4):
            ts = slice(t4*512, (t4+1)*512)
            do_rope(q12[0][:, ts], q12[1][:, ts], qr[0][:, ts], qr[1][:, ts], ts)
        for n in range(16):
            pv = bank()
            for kc in range(4):
                P.op("tensor", "matmul", out=pv[:, 0:128], lhsT=ckv3[:, kc, n*128:(n+1)*128], rhs=wv3[:, kc, :],
                     start=(kc == 0), stop=(kc == 3))
            P.op("vector" if n % 2 else "scalar", "tensor_copy" if n % 2 else "copy", out=Vh[:, n, :], in_=pv[:, 0:128])
        for qb in range(16):
            qs = slice(qb*128, (qb+1)*128)
            nk = (qb + 1) * 128
            S = Ssb[qb % 2]; Pm = Pb[qb % 2]
            for j in range((nk + 511) // 512):
                w = min(512, nk - j*512)
                ks = slice(j*512, j*512 + w)
                pscore = bank()
                P.op("tensor", "matmul", out=pscore[:, 0:w], lhsT=qTn[:, qs], rhs=kTn[:, ks], start=True, stop=False)
                P.op("tensor", "matmul", out=pscore[:, 0:w], lhsT=qr[0][:, qs], rhs=kr[0][:, ks], start=False, stop=False)
                P.op("tensor", "matmul", out=pscore[:, 0:w], lhsT=qr[1][:, qs], rhs=kr[1][:, ks], start=False, stop=True)
                P.op("scalar", "activation", out=S[:, ks], in_=pscore[:, 0:w], func=AF.Identity, scale=SCALE)
            P.op("vector", "tensor_tensor", out=S[:, qb*128:nk], in0=S[:, qb*128:nk], in1=cmask, op=ALU.add)
            c0 = 16 + (qb % 2) * 8
            mx = sm[:, c0:c0+1]; nmx = sm[:, c0+1:c0+2]; rsum = sm[:, c0+2:c0+3]; rrec = sm[:, c0+3:c0+4]
            P.op("vector", "tensor_reduce", out=mx, in_=S[:, 0:nk], axis=AX.X, op=ALU.max)
            P.op("vector", "tensor_scalar", out=nmx, in0=mx, scalar1=-1.0, scalar2=None, op0=ALU.mult)
            P.op("scalar", "activation", out=Pm[:, 0:nk], in_=S[:, 0:nk], func=AF.Exp, bias=nmx, accum_out=rsum)
            for kb in range(qb + 1):
                ptt = ptb[:, (kb % 8)*128:(kb % 8 + 1)*128]
                P.op("tensor", "transpose", out=ptt, in_=Pm[:, kb*128:(kb+1)*128], identity=identb[:])
                PT = PTs[kb % 4]
                P.op("vector" if kb % 2 else "scalar", "tensor_copy" if kb % 2 else "copy", out=PT, in_=ptt)
                P.op("tensor", "matmul", out=pvo[:, 0:128], lhsT=PT, rhs=Vh[:, kb, :], start=(kb == 0), stop=(kb == qb))
            P.op("vector", "reciprocal", out=rrec, in_=rsum)
            ob = osb[qb % 2]
            P.op("vector", "tensor_scalar", out=ob, in0=pvo[:, 0:128], scalar1=rrec, scalar2=None, op0=ALU.mult)
            P.dma("sync", out=yc[qs, h*128:(h+1)*128], in_=ob, slot="ycst%d" % (qb % 2))
    assert o0 <= 16384
    return P.finish()


def host_inputs_F(inp, mod, x1):
    ident = np.eye(128, dtype=np.float32)
    i = np.arange(128)
    cmask = np.where(i[:, None] >= i[None, :], 0.0, -1e9).astype(np.float32)
    cst = np.ascontiguousarray(np.concatenate([ident, cmask], axis=1))
    inv = (10000.0 ** (-np.arange(32, dtype=np.float32) * np.float32(2.0 / 64))).astype(np.float32).reshape(32, 1)
    nbc = np.ascontiguousarray(np.tile(np.concatenate([inp["mla_q_norm"][0], inp["mla_kv_norm"][0]])[None, :], (128, 1)).astype(np.float32))
    wuq_f = inp["mla_w_uq"][0].reshape(512, 16, 192)
    wukv_f = inp["mla_w_ukv"][0].reshape(512, 16, 256)
    maps = []
    for core in range(8):
        b, hh = core // 2, core % 2
        hs = slice(hh*8, hh*8 + 8)
        xT = np.ascontiguousarray(x1[b].T.reshape(16, 128, 2048).transpose(1, 0, 2))
        m = mod[1, b]
        sh1 = m[0:2048]; sc1 = m[2048:4096]
        modc = np.ascontiguousarray(np.concatenate([sc1.reshape(16, 128).T, sh1.reshape(16, 128).T], axis=1))
        wuq = np.ascontiguousarray(wuq_f[:, hs, :].reshape(512, 8 * 192))
        wuk = np.ascontiguousarray(wukv_f[:, hs, 0:128].reshape(512, 1024))
        wuv = np.ascontiguousarray(wukv_f[:, hs, 128:256].reshape(512, 1024))
        posb = np.ascontiguousarray(np.tile(inp["positions"][b][None, :], (32, 1)).astype(np.int32))
        maps.append({"xT": xT, "modc": modc, "win": inp["mla_w_in"][0], "nbc": nbc, "wuq": wuq, "wuk": wuk, "wuv": wuv,
                     "posb": posb, "invc": inv, "cst": cst})
    return maps


def gather_F(results):
    ycat = np.zeros((4, 2048, 2048), np.float32)
    for core in range(8):
        b, hh = core // 2, core % 2
        ycat[b, :, hh*1024:(hh+1)*1024] = results[core]["yc"]
    return ycat


def _run(nc, maps):
    return run_bass_kernel_spmd(nc, maps, core_ids=list(range(8))).results


def kernel(**inputs):
    inp = {k: np.asarray(v) for k, v in inputs.items()}
    c = inp["c"]
    cT = np.ascontiguousarray(c.T.reshape(16, 128, 4).transpose(1, 0, 2))
    mapsA = [{"cT": cT, "aw": np.ascontiguousarray(inp["ada_w"][:, :, i*1536:(i+1)*1536]),
              "ab": np.ascontiguousarray(inp["ada_b"][:, i*1536:(i+1)*1536])} for i in range(8)]
    resA = _run(build_modA(), mapsA)
    mod = np.concatenate([r["mod"] for r in resA], axis=-1)
    del mapsA
    ycat0 = gather_B(_run(build_B(), host_inputs_B(inp, mod)))
    x1 = gather_post(_run(build_post(), host_inputs_post(inp, mod, 0, ycat0, inp["x"])))
    ycat1 = gather_F(_run(build_F(), host_inputs_F(inp, mod, x1)))
    out = gather_post(_run(build_post(), host_inputs_post(inp, mod, 1, ycat1, x1)))
    return np.ascontiguousarray(out.astype(np.float32))
```

```python
from contextlib import ExitStack
import numpy as np
import concourse.bass as bass
import concourse.mybir as mybir
from concourse.bass_utils import run_bass_kernel_spmd

F32 = mybir.dt.float32
BF16 = mybir.dt.bfloat16
I32 = mybir.dt.int32
AF = mybir.ActivationFunctionType
ALU = mybir.AluOpType
AX = mybir.AxisListType
ENGS = ("sync", "scalar", "vector", "gpsimd", "tensor")
_DTSZ = {"float32": 4, "bfloat16": 2, "int32": 4, "float16": 2, "uint32": 4, "int16": 2,
         "uint16": 2, "int8": 1, "uint8": 1, "float32r": 4}
WRITE_KW = ("out", "accum_out", "out_max", "out_indices", "ap")


def _dtsz(dt):
    s = str(dt).split(".")[-1]
    return _DTSZ.get(s, 4)


def _rect(ap):
    name = ap.tensor.name
    pat = ap.ap
    off = ap.offset
    esz = _dtsz(ap.dtype)
    space = str(ap.space)
    if not isinstance(off, int):
        return (name, 0, 1 << 30, 0, 1 << 40)
    if "DRAM" in space or "Dram" in space:
        ext = sum((c - 1) * abs(s) for s, c in pat) + 1
        return (name, 0, 1, off * esz, (off + ext) * esz)
    if "PSUM" in space.upper():
        return (name, 0, 128, 0, 1 << 20)
    ps, pc = pat[0]
    if ps == 0:
        ps = 1 << 30
    p0 = off // ps if ps < (1 << 30) else 0
    fo = off - p0 * ps if ps < (1 << 30) else off
    ext = sum((c - 1) * abs(s) for s, c in pat[1:]) + 1
    return (name, p0, p0 + pc, fo * esz, (fo + ext) * esz)


def _ovl(a, b):
    return a[1] < b[2] and b[1] < a[2] and a[3] < b[4] and b[3] < a[4]


def _contains(a, b):
    return a[1] <= b[1] and b[2] <= a[2] and a[3] <= b[3] and b[4] <= a[4]


class Prog:
    def __init__(self, nc, serialize=False):
        self.nc = nc
        self.es = ExitStack()
        self.ops = {e: [] for e in ENGS}
        self.cnt = {e: 0 for e in ENGS}
        self.esem = {}
        self.slot_cnt = {}
        self.slot_sem = {}
        self.slot_waiters = {}
        self.recs = {}
        self.waited = {e: {} for e in ENGS}
        self.serialize = serialize
        self.last_event = None
        self.nuid = 0
        for e in ENGS:
            self.esem[e] = self.es.enter_context(nc.semaphore("es_" + e))

    def sb(self, name, shape, dt=F32):
        return self.es.enter_context(self.nc.sbuf_tensor(name, list(shape), dt))

    def ps(self, name, shape, dt=F32):
        return self.es.enter_context(self.nc.psum_tensor(name, list(shape), dt))

    def dram(self, name, shape, dt=F32, kind="Internal"):
        return self.nc.dram_tensor(name, list(shape), dt, kind=kind).ap()

    def _slot_sem(self, slot):
        if slot not in self.slot_sem:
            self.slot_sem[slot] = self.es.enter_context(self.nc.semaphore("ds_%d" % len(self.slot_sem)))
            self.slot_cnt[slot] = 0
            self.slot_waiters[slot] = {}
        return self.slot_sem[slot]

    def _deps(self, reads, writes, eng, is_pe):
        deps = []
        for ap in reads:
            r = _rect(ap)
            psum = r[4] == (1 << 20)
            for (rr, kind, ev, e2) in self.recs.get(r[0], []):
                if _ovl(r, rr) and (kind == "w" or (psum and e2 != eng)):
                    deps.append((ev, e2))
        for ap in writes:
            r = _rect(ap)
            for (rr, kind, ev, e2) in self.recs.get(r[0], []):
                if _ovl(r, rr):
                    deps.append((ev, e2))
        return deps

    def _record(self, reads, writes, ev, eng):
        for ap in writes:
            r = _rect(ap)
            lst = self.recs.setdefault(r[0], [])
            lst[:] = [x for x in lst if not _contains(r, x[0])]
            lst.append((r, "w", ev, eng))
        for ap in reads:
            r = _rect(ap)
            lst = self.recs.setdefault(r[0], [])
            lst[:] = [x for x in lst if not (x[1] == "r" and x[3] == eng and x[0] == r
                                             and x[2][0] == ev[0])]
            lst.append((r, "r", ev, eng))

    def _mkwaits(self, eng, deps, is_pe):
        waits = {}
        for (ev, e2) in deps:
            if ev is None:
                continue
            key, val = ev
            if is_pe and key == ("e", "tensor"):
                continue
            if key[0] == "d":
                val = self.slot_cnt[key[1]]
            if self.waited[eng].get(key, 0) >= val:
                continue
            waits[key] = max(waits.get(key, 0), val)
        for key, val in waits.items():
            self.waited[eng][key] = val
        return waits

    def op(self, eng, method, *args, reads=(), writes=(), **kw):
        aps_r = list(reads)
        aps_w = list(writes)
        for k, v in kw.items():
            if hasattr(v, "tensor") and hasattr(v, "ap"):
                (aps_w if k in WRITE_KW else aps_r).append(v)
        for v in args:
            if hasattr(v, "tensor") and hasattr(v, "ap"):
                aps_r.append(v)
        is_pe = eng == "tensor"
        deps = self._deps(aps_r, aps_w, eng, is_pe)
        if self.serialize and self.last_event is not None:
            deps.append((self.last_event, None))
        waits = self._mkwaits(eng, deps, is_pe)
        self.cnt[eng] += 1
        ev = (("e", eng), self.cnt[eng])
        for key in waits:
            if key[0] == "d":
                self.slot_waiters[key[1]][eng] = ev
        self._record(aps_r, aps_w, ev, eng)
        self.ops[eng].append(("c", method, args, kw, waits, ev))
        self.last_event = ev
        return ev

    def dma(self, eng, out, in_, slot=None, indirect=None, **kw):
        if slot is None:
            slot = out.tensor.name
        if eng == "gpsimd":
            slot = "sw_" + slot
        self._slot_sem(slot)
        aps_r = [in_]
        aps_w = [out]
        if indirect is not None:
            aps_r.append(indirect["idx"])
        deps = self._deps(aps_r, aps_w, eng, False)
        for e2, ev2 in self.slot_waiters[slot].items():
            deps.append((ev2, e2))
        if self.serialize and self.last_event is not None:
            deps.append((self.last_event, None))
        waits = self._mkwaits(eng, deps, False)
        self.slot_cnt[slot] += 16
        ev = (("d", slot), self.slot_cnt[slot])
        for key in waits:
            if key[0] == "d" and key[1] != slot:
                self.slot_waiters[key[1]][eng + "_q"] = ev
        self._record(aps_r, aps_w, ev, "dma_" + eng)
        self.ops[eng].append(("d", None, (out, in_, indirect), kw, waits, ev))
        self.last_event = ev
        return ev

    def dyn(self, eng, fn, reads=(), writes=(), slot=None):
        if eng == "gpsimd":
            slot = "sw_" + slot
        self._slot_sem(slot)
        deps = self._deps(list(reads), list(writes), eng, False)
        for e2, ev2 in self.slot_waiters[slot].items():
            deps.append((ev2, e2))
        waits = self._mkwaits(eng, deps, False)
        self.slot_cnt[slot] += 16
        ev = (("d", slot), self.slot_cnt[slot])
        for key in waits:
            if key[0] == "d" and key[1] != slot:
                self.slot_waiters[key[1]][eng + "_q"] = ev
        self._record(list(reads), list(writes), ev, "dma_" + eng)
        self.ops[eng].append(("f", fn, None, None, waits, ev))
        return ev

    def _sem_of(self, key):
        return self.esem[key[1]] if key[0] == "e" else self.slot_sem[key[1]]

    def _emit(self, engname, e):
        for (kind, method, args, kw, waits, ev) in self.ops[engname]:
            for key, val in waits.items():
                e.wait_ge(self._sem_of(key), val)
            if kind == "c":
                ins = getattr(e, method)(*args, **kw)
                ins.then_inc(self.esem[engname], 1)
            elif kind == "f":
                ins = method(e)
                ins.then_inc(self._sem_of(ev[0]), 16)
            else:
                out, in_, ind = args
                if ind is None:
                    ins = e.dma_start(out=out, in_=in_, **kw)
                else:
                    off = bass.IndirectOffsetOnAxis(ap=ind["idx"], axis=0)
                    if ind["mode"] == "gather":
                        ins = e.indirect_dma_start(out=out, out_offset=None, in_=in_, in_offset=off, **kw)
                    else:
                        ins = e.indirect_dma_start(out=out, out_offset=off, in_=in_, in_offset=None, **kw)
                ins.then_inc(self._sem_of(ev[0]), 16)
        if engname == "sync":
            for slot, c in self.slot_cnt.items():
                if c:
                    e.wait_ge(self.slot_sem[slot], c)
            for en in ENGS:
                if en != "sync" and self.cnt[en]:
                    e.wait_ge(self.esem[en], self.cnt[en])

    def finish(self):
        nc = self.nc
        with nc.Block() as block:
            @block.sync
            def _(e):
                self._emit("sync", e)

            @block.scalar
            def _(e):
                self._emit("scalar", e)

            @block.vector
            def _(e):
                self._emit("vector", e)

            @block.gpsimd
            def _(e):
                self._emit("gpsimd", e)

            @block.tensor
            def _(e):
                self._emit("tensor", e)
        self.es.close()
        return nc


def build_modA():
    nc = bass.Bass("TRN2", target_bir_lowering=False)
    P = Prog(nc)
    cT = nc.dram_tensor("cT", [128, 16, 4], F32, kind="ExternalInput").ap()
    aw = nc.dram_tensor("aw", [2, 2048, 1536], F32, kind="ExternalInput").ap()
    ab = nc.dram_tensor("ab", [2, 1536], F32, kind="ExternalInput").ap()
    out = nc.dram_tensor("mod", [2, 4, 1536], F32, kind="ExternalOutput").ap()
    ct = P.sb("ct", [128, 64]); cs = P.sb("cs", [128, 64])
    wb = [P.sb("w%d" % i, [128, 4, 1536]) for i in range(2)]
    bias = P.sb("bias", [4, 2, 1536]); res = P.sb("res", [4, 2, 1536])
    pp = [P.ps("pp%d" % i, [128, 2048]) for i in range(2)]
    P.dma("sync", out=ct[:], in_=cT.rearrange("p k b -> p (k b)"))
    for l in range(2):
        for b in range(4):
            P.dma("sync", out=bias[b:b+1, l, :], in_=ab[l:l+1, :])
    P.op("scalar", "activation", out=cs[:], in_=ct[:], func=AF.Silu)
    it = 0
    for l in range(2):
        for kg in range(4):
            w = wb[it % 2]; it += 1
            P.dma("sync", out=w[:], in_=aw[l, kg*512:(kg+1)*512, :].rearrange("(k p) n -> p k n", p=128))
            for kk in range(4):
                k = kg*4+kk
                for j in range(3):
                    P.op("tensor", "matmul", out=pp[l][0:4, j*512:(j+1)*512], lhsT=cs[:, k*4:(k+1)*4],
                         rhs=w[:, kk, j*512:(j+1)*512], start=(k == 0), stop=(k == 15))
        P.op("vector", "tensor_tensor", out=res[:, l, :], in0=pp[l][0:4, 0:1536], in1=bias[:, l, :], op=ALU.add)
        P.dma("sync", out=out[l], in_=res[:, l, :])
    return P.finish()


import numpy as np

NT = 16


def consts_np():
    i = np.arange(128)
    ident = np.eye(128, dtype=np.float32)
    U = (i[:, None] <= i[None, :]).astype(np.float32)
    negmask = -(i[:, None] > i[None, :]).astype(np.float32)
    maskT = (i[:, None] <= i[None, :]).astype(np.float32)
    ones = np.ones((128, 128), np.float32)
    return np.ascontiguousarray(np.concatenate([ident, U, negmask, maskT, ones], axis=1))


def build_B(stop=None):
    nc = bass.Bass("TRN2", target_bir_lowering=False)
    import os
    P = Prog(nc, serialize=bool(os.environ.get("SER")))
    xT = nc.dram_tensor("xT", [128, 16, 2048], F32, kind="ExternalInput").ap()
    modc = nc.dram_tensor("modc", [128, 32], F32, kind="ExternalInput").ap()
    wf = nc.dram_tensor("wf", [8, 2048, 384], F32, kind="ExternalInput").ap()
    wz = nc.dram_tensor("wz", [2048, 512], F32, kind="ExternalInput").ap()
    wba = nc.dram_tensor("wba", [128, 16, 8], F32, kind="ExternalInput").ap()
    cw = nc.dram_tensor("cw", [128, 12, 4], F32, kind="ExternalInput").ap()
    scw = nc.dram_tensor("scw", [128, 4, 3], F32, kind="ExternalInput").ap()
    hv = nc.dram_tensor("hv", [128, 8], F32, kind="ExternalInput").ap()
    nw = nc.dram_tensor("nw", [128, 128], F32, kind="ExternalInput").ap()
    cst = nc.dram_tensor("cst", [128, 640], F32, kind="ExternalInput").ap()
    ya = nc.dram_tensor("ya", [2048, 512], F32, kind="ExternalOutput").ap()
    ybT = nc.dram_tensor("ybT", [512, 2048], F32, kind="ExternalOutput").ap()
    raw = P.dram("rawscr", [24, 128, 2048], F32)

    csb = P.sb("csb", [128, 640])
    ident, U, negmask, maskT, ones = (csb[:, i*128:(i+1)*128] for i in range(5))
    small = P.sb("small", [128, 512])
    mod_sb = small[:, 0:32]; scp1 = small[:, 32:48]
    hv_sb = small[:, 48:56]; ea = small[:, 56:60]
    cw_sb = P.sb("cw_sb", [128, 48]); scw_sb = P.sb("scw_sb", [128, 12])
    nw_sb = P.sb("nw_sb", [128, 128])
    wba_sb = P.sb("wba_sb", [128, 128])
    ba = P.sb("ba", [128, NT * 8])
    g_all = P.sb("g_all", [128, NT * 4]); beta_all = P.sb("beta_all", [128, NT * 4])
    gc_all = P.sb("gc_all", [128, NT * 4])
    sz = P.sb("sz", [128, NT * 512], BF16)
    arena = P.sb("arena", [128, 36 * 1024])
    psb = [P.ps("ps%d" % i, [128, 512]) for i in range(8)]
    pq = [0]

    def psq():
        i = pq[0]; pq[0] = (i + 1) % 32
        return psb[i // 4][:, (i % 4) * 128:(i % 4 + 1) * 128]
    pb = [0]

    def psbank():
        i = pb[0]; pb[0] = (i + 1) % 8
        pq[0] = 0
        return psb[i]

    cb = P.sb("cb", [128, 4])
    P.op("vector", "memset", ap=cb[:, 0:1], constant=1e-6)
    P.op("vector", "memset", ap=cb[:, 1:2], constant=1.0)
    P.dma("sync", out=csb[:], in_=cst)
    P.dma("sync", out=mod_sb, in_=modc)
    P.dma("sync", out=hv_sb, in_=hv)
    P.dma("sync", out=cw_sb[:], in_=cw.rearrange("p a b -> p (a b)"))
    P.dma("sync", out=scw_sb[:], in_=scw.rearrange("p a b -> p (a b)"))
    P.dma("sync", out=nw_sb[:], in_=nw)
    P.dma("sync", out=wba_sb[:], in_=wba.rearrange("p a b -> p (a b)"))
    P.op("vector", "tensor_scalar", out=scp1, in0=mod_sb[:, 0:16], scalar1=1.0, scalar2=None, op0=ALU.add)
    P.op("scalar", "activation", out=ea, in_=hv_sb[:, 0:4], func=AF.Exp)

    hinT = arena[:, 0:16384].bitcast(BF16).rearrange("p (k t) -> p k t", k=16)
    xt = [arena[:, 16384 + i*4096:16384 + (i+1)*4096].rearrange("p (k t) -> p k t", k=16) for i in range(1)]
    hf = arena[:, 20480:24576].rearrange("p (k t) -> p k t", k=16)
    wbuf = [arena[:, 24576 + i*3072:24576 + (i+1)*3072].bitcast(BF16).rearrange("p (k n) -> p k n", k=16)
            for i in range(1)]
    wzb = arena[:, 24576 + 3072:24576 + 3072 + 4096].bitcast(BF16).rearrange("p (k n) -> p k n", k=16)
    ev = [small[:, 64:64], ]
    evb = P.sb("evb", [128, 2 * 512])
    for tt in range(8):
        t0 = tt * 256
        P.dma("sync", out=xt[0], in_=xT[:, :, t0:t0 + 256])
        for k in range(16):
            P.op("scalar", "activation", out=hf[:, k, :], in_=xt[0][:, k, :], func=AF.Identity,
                 scale=scp1[:, k:k+1], bias=mod_sb[:, 16 + k:17 + k])
            P.op("vector" if k % 2 else "gpsimd", "tensor_copy", out=hinT[:, k, t0:t0 + 256], in_=hf[:, k, :])
        for sub in range(2):
            n = tt * 2 + sub
            pp = psbank()
            for k in range(16):
                P.op("tensor", "matmul", out=pp[:, 0:8], lhsT=hf[:, k, sub*128:(sub+1)*128],
                     rhs=wba_sb[:, k*8:(k+1)*8], start=(k == 0), stop=(k == 15))
            P.op("vector", "tensor_copy", out=ba[:, n*8:(n+1)*8], in_=pp[:, 0:8])
    ba3 = ba[:, :].rearrange("p (n c) -> p n c", c=8)
    b3 = beta_all[:, :].rearrange("p (n c) -> p n c", c=4)
    g3 = g_all[:, :].rearrange("p (n c) -> p n c", c=4)
    P.op("scalar", "activation", out=b3, in_=ba3[:, :, 0:4], func=AF.Sigmoid)
    for n in range(NT):
        P.op("vector", "tensor_tensor", out=g3[:, n, :], in0=ba3[:, n, 4:8], in1=hv_sb[:, 4:8], op=ALU.add)
    P.op("scalar", "activation", out=g_all[:], in_=g_all[:], func=AF.Exp)
    P.op("scalar", "activation", out=g_all[:], in_=g_all[:], func=AF.Ln, bias=cb[:, 1:2])
    for n in range(NT):
        P.op("vector", "scalar_tensor_tensor", out=g3[:, n, :], in0=g3[:, n, :], scalar=-1.0, in1=ea,
             op0=ALU.mult, op1=ALU.mult)
    pp = psbank()
    P.op("tensor", "matmul", out=pp[:, 0:64], lhsT=U, rhs=g_all[:], start=True, stop=True)
    P.op("vector", "tensor_copy", out=gc_all[:], in_=pp[:, 0:64])

    if stop == "P0":
        return P.finish()
    P.dma("gpsimd", out=wzb, in_=wz.rearrange("(k p) n -> p k n", p=128))
    for n in range(NT):
        pp = psbank()
        for k in range(16):
            P.op("tensor", "matmul", out=pp[:, :], lhsT=hinT[:, k, n*128:(n+1)*128], rhs=wzb[:, k, :],
                 start=(k == 0), stop=(k == 15))
        P.op("scalar", "activation", out=sz[:, n*512:(n+1)*512], in_=pp[:, :], func=AF.Silu)
    for gi in range(8):
        P.dma("gpsimd", out=wbuf[0], in_=wf[gi].rearrange("(k p) n -> p k n", p=128))
        for c3 in range(3):
            for t4 in range(4):
                pp = psbank()
                for k in range(16):
                    P.op("tensor", "matmul", out=pp[:, :], lhsT=wbuf[0][:, k, c3*128:(c3+1)*128],
                         rhs=hinT[:, k, t4*512:(t4+1)*512], start=(k == 0), stop=(k == 15))
                e = evb[:, (t4 % 2)*512:(t4 % 2 + 1)*512]
                P.op("vector" if t4 % 2 else "scalar", "tensor_copy" if t4 % 2 else "copy", out=e, in_=pp[:, :])
                P.dma("sync", out=raw[gi*3 + c3, :, t4*512:(t4+1)*512], in_=e, slot="rawst%d" % (t4 % 2))

    if stop == "P1":
        return P.finish()
    A = arena
    rawb = A[:, 0:3 * 2052].rearrange("p (c t) -> p c t", c=3)
    QKV = A[:, 6400:6400 + 3 * 2048].rearrange("p (c t) -> p c t", c=3)
    o0 = 12544
    u_all = A[:, o0:o0 + 2048].rearrange("p (n e) -> p n e", n=NT); o0 += 2048
    wT_all = A[:, o0:o0 + 2048]; o0 += 2048
    qkT_all = A[:, o0:o0 + 2048].rearrange("p (n e) -> p n e", n=NT); o0 += 2048
    QgT_all = A[:, o0:o0 + 2048]; o0 += 2048
    kdec_all = A[:, o0:o0 + 2048].rearrange("p (n e) -> p n e", n=NT); o0 += 2048
    ya_h = A[:, o0:o0 + 2048].rearrange("p (n e) -> p n e", n=NT); o0 += 2048
    nz = A[:, o0:o0 + 2048].rearrange("p (n e) -> p n e", n=NT); o0 += 2048
    tmpc = A[:, o0:o0 + 2052]; o0 += 2052
    sq = A[:, o0:o0 + 512]; o0 += 512
    rs = A[:, o0:o0 + 512]; o0 += 512
    egl_all = A[:, o0:o0 + 16]; o0 += 16
    Sst = [A[:, o0 + i*128:o0 + (i+1)*128] for i in range(2)]; o0 += 256
    NSL = 2
    W = []
    for s in range(NSL):
        names = ["gb", "t1", "D1", "t2", "D2", "Egb", "cols", "Nm", "NTm", "Pa", "Pb", "PTa", "PTb", "XTa", "XTb",
                 "vb", "kbg", "t3", "t4"]
        d = {}
        for nm in names:
            d[nm] = A[:, o0:o0 + 128]; o0 += 128
        W.append(d)
    assert o0 <= 36 * 1024, o0
    P.op("vector", "memset", ap=rawb[:, :, 0:4], constant=0.0)

    def phase1_steps(h, n, s):
        w = W[s]; tok = slice(n*128, (n+1)*128); col = n*4 + h
        gcol = g_all[:, col:col+1]; gccol = gc_all[:, col:col+1]; bcol = beta_all[:, col:col+1]
        QT = QKV[:, 0, tok]; KT = QKV[:, 1, tok]; VT = QKV[:, 2, tok]
        st = {}

        def s1():
            P.op("vector", "tensor_scalar", out=w["gb"], in0=ones, scalar1=gcol, scalar2=None, op0=ALU.mult)
            st["G"] = psq()
            P.op("tensor", "matmul", out=st["G"], lhsT=w["gb"], rhs=U, start=True, stop=True)
            st["KK"] = psq(); st["QK"] = psq()
            P.op("tensor", "matmul", out=st["KK"], lhsT=KT, rhs=KT, start=True, stop=True)
            P.op("tensor", "matmul", out=st["QK"], lhsT=KT, rhs=QT, start=True, stop=True)

        def s2():
            G = st["G"]
            P.op("vector", "tensor_scalar", out=w["t1"], in0=G, scalar1=gccol, scalar2=0.0, op0=ALU.subtract, op1=ALU.max)
            P.op("scalar", "activation", out=w["D1"], in_=w["t1"], func=AF.Exp, scale=-1.0)
            P.op("vector", "tensor_scalar", out=w["t2"], in0=G, scalar1=gccol, scalar2=0.0, op0=ALU.subtract, op1=ALU.min)
            P.op("scalar", "activation", out=w["D2"], in_=w["t2"], func=AF.Exp)
            P.op("scalar", "activation", out=w["Egb"], in_=G, func=AF.Exp)
            c = w["cols"]
            P.op("scalar", "activation", out=c[:, 0:1], in_=gccol, func=AF.Exp)
            P.op("vector", "tensor_tensor", out=c[:, 1:2], in0=c[:, 0:1], in1=bcol, op=ALU.mult)
            P.op("vector", "tensor_copy", out=c[:, 3:4], in_=G[:, 127:128])
            P.op("vector", "tensor_tensor", out=c[:, 4:5], in0=c[:, 3:4], in1=gccol, op=ALU.subtract)
            P.op("scalar", "activation", out=c[:, 2:3], in_=c[:, 4:5], func=AF.Exp)
            P.op("vector", "tensor_copy", out=egl_all[:, n:n+1], in_=w["Egb"][:, 127:128])

        def s3():
            P.op("vector", "scalar_tensor_tensor", out=w["t3"], in0=st["KK"], scalar=bcol, in1=w["D1"],
                 op0=ALU.mult, op1=ALU.mult)
            P.op("gpsimd", "tensor_tensor", out=w["Nm"], in0=w["t3"], in1=negmask, op=ALU.mult)
            P.op("vector", "tensor_tensor", out=w["t4"], in0=st["QK"], in1=w["D2"], op=ALU.mult)
            P.op("gpsimd", "tensor_tensor", out=qkT_all[:, n, :], in0=w["t4"], in1=maskT, op=ALU.mult)
            P.op("gpsimd", "tensor_tensor", out=QgT_all[:, tok], in0=QT, in1=w["Egb"], op=ALU.mult)
            st["NTp"] = psq()
            P.op("tensor", "transpose", out=st["NTp"], in_=w["Nm"], identity=ident)
            st["Kt"] = psq(); st["Vt"] = psq()
            P.op("tensor", "transpose", out=st["Kt"], in_=KT, identity=ident)
            P.op("tensor", "transpose", out=st["Vt"], in_=VT, identity=ident)

        def s4():
            P.op("scalar", "copy", out=w["NTm"], in_=st["NTp"])
            P.op("vector", "tensor_tensor", out=w["XTa"], in0=st["NTp"], in1=ident, op=ALU.add)
            c = w["cols"]
            P.op("scalar", "activation", out=w["kbg"], in_=st["Kt"], func=AF.Identity, scale=c[:, 1:2])
            P.op("vector", "tensor_scalar", out=kdec_all[:, n, :], in0=st["Kt"], scalar1=c[:, 2:3], scalar2=None, op0=ALU.mult)
            P.op("scalar", "activation", out=w["vb"], in_=st["Vt"], func=AF.Identity, scale=bcol)
            st["P"] = w["Nm"]; st["PT"] = w["NTm"]; st["XT"] = w["XTa"]; st["lvl"] = 0

        def sq_a():
            lvl = st["lvl"]
            st["Pp"] = psq()
            P.op("tensor", "matmul", out=st["Pp"], lhsT=st["PT"], rhs=st["P"], start=True, stop=True)
            if lvl < 5:
                st["PTp"] = psq()
                P.op("tensor", "matmul", out=st["PTp"], lhsT=st["P"], rhs=st["PT"], start=True, stop=True)

        def sq_b():
            lvl = st["lvl"]
            Pn = w["Pa"] if lvl % 2 == 0 else w["Pb"]
            PTn = w["PTa"] if lvl % 2 == 0 else w["PTb"]
            P.op("scalar", "copy", out=Pn, in_=st["Pp"])
            if lvl < 5:
                P.op("vector", "tensor_copy", out=PTn, in_=st["PTp"])
            st["Xp"] = psq()
            P.op("tensor", "matmul", out=st["Xp"], lhsT=Pn, rhs=st["XT"], start=True, stop=True)
            st["P"] = Pn; st["PT"] = PTn

        def sq_c():
            Xn = w["XTb"] if st["XT"] is w["XTa"] else w["XTa"]
            P.op("vector", "tensor_tensor", out=Xn, in0=st["Xp"], in1=st["XT"], op=ALU.add)
            st["XT"] = Xn; st["lvl"] += 1

        def s9():
            pu = psq(); pw = psq()
            P.op("tensor", "matmul", out=pu, lhsT=st["XT"], rhs=w["vb"], start=True, stop=True)
            P.op("tensor", "matmul", out=pw, lhsT=w["kbg"], rhs=st["XT"], start=True, stop=True)
            P.op("scalar", "copy", out=u_all[:, n, :], in_=pu)
            P.op("vector", "tensor_copy", out=wT_all[:, tok], in_=pw)
        steps = [s1, s2, s3, s4]
        for _ in range(6):
            steps += [sq_a, sq_b, sq_c]
        steps.append(s9)
        return steps

    for h in range(4):
        for c3 in range(3):
            P.dma("sync", out=rawb[:, c3, 4:2052], in_=raw[h*3 + c3])
        for c3 in range(3):
            ch = h*3 + c3
            eng = "vector"
            P.op(eng, "tensor_scalar", out=tmpc[:, 0:2048], in0=rawb[:, c3, 1:2049], scalar1=cw_sb[:, ch*4:ch*4+1],
                 scalar2=None, op0=ALU.mult)
            for j in range(1, 4):
                P.op(eng, "scalar_tensor_tensor", out=tmpc[:, 0:2048], in0=rawb[:, c3, 1+j:2049+j],
                     scalar=cw_sb[:, ch*4+j:ch*4+j+1], in1=tmpc[:, 0:2048], op0=ALU.mult, op1=ALU.add)
            P.op("scalar", "activation", out=QKV[:, c3, :], in_=tmpc[:, 0:2048], func=AF.Silu)
            if c3 < 2:
                for t4 in range(4):
                    ts = slice(t4*512, (t4+1)*512)
                    P.op("scalar", "activation", out=sq, in_=QKV[:, c3, ts], func=AF.Square)
                    pp = psbank()
                    P.op("tensor", "matmul", out=pp[:, :], lhsT=ones, rhs=sq, start=True, stop=True)
                    P.op("scalar", "activation", out=rs, in_=pp[:, :], func=AF.Ln, bias=cb[:, 0:1])
                    P.op("scalar", "activation", out=rs, in_=rs, func=AF.Exp, scale=-0.5)
                    P.op("vector", "scalar_tensor_tensor", out=QKV[:, c3, ts], in0=rs, scalar=(128 ** -0.5 if c3 == 0 else 1.0),
                         in1=QKV[:, c3, ts], op0=ALU.mult, op1=ALU.mult)
        if stop == "G0":
            return P.finish()
        for n in range(NT):
            P.op("gpsimd", "tensor_tensor", out=nz[:, n, :], in0=sz[:, n*512 + h*128:n*512 + (h+1)*128], in1=nw_sb[:], op=ALU.mult)
        for n0 in range(0, NT, NSL):
            lists = [phase1_steps(h, n0 + s, s) for s in range(NSL)]
            for i in range(len(lists[0])):
                if stop is not None and stop.startswith("S") and i >= int(stop[1:]):
                    return P.finish()
                for s in range(NSL):
                    lists[s][i]()
        if stop == "G1":
            return P.finish()
        P.op("vector", "memset", ap=Sst[0], constant=0.0)
        for n in range(NT):
            tok = slice(n*128, (n+1)*128)
            S = Sst[n % 2]; Sn = Sst[(n + 1) % 2]
            pws = psq()
            P.op("tensor", "matmul", out=pws, lhsT=wT_all[:, tok], rhs=S, start=True, stop=True)
            vnew = W[0]["t3"] if n % 2 == 0 else W[1]["t3"]
            P.op("vector", "tensor_tensor", out=vnew, in0=u_all[:, n, :], in1=pws, op=ALU.subtract)
            po = psq(); pd = psq()
            P.op("tensor", "matmul", out=po, lhsT=QgT_all[:, tok], rhs=S, start=True, stop=False)
            P.op("tensor", "matmul", out=po, lhsT=qkT_all[:, n, :], rhs=vnew, start=False, stop=True)
            P.op("tensor", "matmul", out=pd, lhsT=kdec_all[:, n, :], rhs=vnew, start=True, stop=True)
            P.op("vector", "scalar_tensor_tensor", out=Sn, in0=S, scalar=egl_all[:, n:n+1], in1=pd, op0=ALU.mult, op1=ALU.add)
            ssq = W[n % 2]["cols"][:, 8:9]; rstd = W[n % 2]["cols"][:, 9:10]
            junk = W[n % 2]["t4"]
            P.op("scalar", "activation", out=junk, in_=po, func=AF.Square, accum_out=ssq)
            P.op("scalar", "activation", out=rstd, in_=ssq, func=AF.Ln, scale=1.0 / 128, bias=cb[:, 0:1])
            P.op("scalar", "activation", out=rstd, in_=rstd, func=AF.Exp, scale=-0.5)
            P.op("vector", "scalar_tensor_tensor", out=ya_h[:, n, :], in0=po, scalar=rstd, in1=nz[:, n, :], op0=ALU.mult, op1=ALU.mult)
        P.dma("sync", out=ya[:, h*128:(h+1)*128].rearrange("(n p) e -> p n e", p=128), in_=ya_h, slot="yast")

        if stop == "G2":
            return P.finish()
    bg = rawb
    for c in range(4):
        for c3 in range(3):
            P.dma("sync", out=rawb[:, c3, 4:2052], in_=raw[12 + c*3 + c3])
        P.op("vector", "memset", ap=tmpc[:, 0:4], constant=0.0)
        P.op("vector", "tensor_tensor", out=tmpc[:, 4:2052], in0=rawb[:, 1, 4:2052], in1=rawb[:, 2, 4:2052], op=ALU.mult)
        acc = QKV[:, 0, :]
        P.op("vector", "tensor_scalar", out=acc, in0=tmpc[:, 2:2050], scalar1=scw_sb[:, c*3:c*3+1], scalar2=None, op0=ALU.mult)
        for j in range(1, 3):
            P.op("vector", "scalar_tensor_tensor", out=acc, in0=tmpc[:, 2+j:2050+j], scalar=scw_sb[:, c*3+j:c*3+j+1],
                 in1=acc, op0=ALU.mult, op1=ALU.add)
        ob = QKV[:, 1 + c % 2, :]
        P.op("gpsimd", "tensor_tensor", out=ob, in0=acc, in1=rawb[:, 0, 4:2052], op=ALU.mult)
        P.dma("sync", out=ybT[c*128:(c+1)*128, :], in_=ob, slot="ybst%d" % (c % 2))
    return P.finish()


def host_inputs_B(inp, mod):
    w_in = inp["hyb_w_in"][0]
    cst = consts_np()
    maps = []
    G = 1024
    for core in range(8):
        b, hh = core // 2, core % 2
        xT = np.ascontiguousarray(inp["x"][b].T.reshape(16, 128, 2048).transpose(1, 0, 2))
        sh1 = mod[0, b, 0:2048]; sc1 = mod[0, b, 2048:4096]
        modc = np.ascontiguousarray(np.concatenate([sc1.reshape(16, 128).T, sh1.reshape(16, 128).T], axis=1))
        groups = []
        for hl in range(4):
            h = hh*4 + hl
            cols = np.concatenate([np.arange(h*128, (h+1)*128) + o for o in (0, G, 2*G)])
            groups.append(w_in[:, cols])
        base = 4*G + 16
        for cl in range(4):
            c0 = hh*512 + cl*128
            cols = np.concatenate([np.arange(c0, c0+128) + base + o for o in (0, G, 2*G)])
            groups.append(w_in[:, cols])
        wf = np.ascontiguousarray(np.stack(groups))
        wz = np.ascontiguousarray(w_in[:, 3*G + hh*512:3*G + (hh+1)*512])
        bcols = np.concatenate([4*G + hh*4 + np.arange(4), 4*G + 8 + hh*4 + np.arange(4)])
        wba = np.ascontiguousarray(w_in[:, bcols].reshape(16, 128, 8).transpose(1, 0, 2))
        cwf = inp["gdn_conv_w"][0]
        cw = np.zeros((128, 12, 4), np.float32)
        for hl in range(4):
            h = hh*4 + hl
            for c3 in range(3):
                cw[:, hl*3 + c3, :] = cwf[:, c3*G + h*128:c3*G + (h+1)*128].T
        scf = inp["sc_conv_w"][0]
        scw = np.zeros((128, 4, 3), np.float32)
        for cl in range(4):
            c0 = hh*512 + cl*128
            scw[:, cl, :] = scf[:, c0:c0+128].T
        hv = np.tile(np.concatenate([inp["gdn_a_log"][0][hh*4:hh*4+4], inp["gdn_dt_bias"][0][hh*4:hh*4+4]])[None, :], (128, 1)).astype(np.float32)
        nw = np.tile(inp["gdn_norm_w"][0][None, :], (128, 1)).astype(np.float32)
        maps.append({"xT": xT, "modc": modc, "wf": wf, "wz": wz, "wba": wba, "cw": cw, "scw": scw,
                     "hv": np.ascontiguousarray(hv), "nw": np.ascontiguousarray(nw), "cst": cst})
    return maps


def gather_B(results):
    ycat = np.zeros((4, 2048, 2048), np.float32)
    for core in range(8):
        b, hh = core // 2, core % 2
        ycat[b, :, hh*512:(hh+1)*512] = results[core]["ya"]
        ycat[b, :, 1024 + hh*512:1024 + (hh+1)*512] = results[core]["ybT"].T
    return ycat


import numpy as np

DN_ALPHA = 4.0 ** 0.25
D = 2048


def build_post(n_experts=64, precast=False):
    NB = 80
    nc = bass.Bass("TRN2", target_bir_lowering=False)
    P = Prog(nc)
    ycT = nc.dram_tensor("ycT", [128, 16, 1024], F32, kind="ExternalInput").ap()
    xin = nc.dram_tensor("xin", [1024, 2048], F32, kind="ExternalInput").ap()
    wout = nc.dram_tensor("wout", [2048, 2048], F32, kind="ExternalInput").ap()
    bc1 = nc.dram_tensor("bc1", [128, 3, 2048], F32, kind="ExternalInput").ap()
    bc2 = nc.dram_tensor("bc2", [128, 3, 2048], F32, kind="ExternalInput").ap()
    colv = nc.dram_tensor("colv", [128, 32], F32, kind="ExternalInput").ap()
    wr = nc.dram_tensor("wr", [128, 16, 72], F32, kind="ExternalInput").ap()
    br = nc.dram_tensor("br", [128, 72], F32, kind="ExternalInput").ap()
    wg = nc.dram_tensor("wg", [64 * 128 * 4, 2048], F32, kind="ExternalInput").ap()
    wu = nc.dram_tensor("wu", [64 * 128 * 4, 2048], F32, kind="ExternalInput").ap()
    wd = nc.dram_tensor("wd", [64 * 128 * 4, 2048], F32, kind="ExternalInput").ap()
    idn = nc.dram_tensor("idn", [128, 128], F32, kind="ExternalInput").ap()
    xo = nc.dram_tensor("xo", [1024, 2048], F32, kind="ExternalOutput").ap()
    thr = nc.dram_tensor("thr", [128, 260], F32, kind="ExternalInput").ap()
    x1s = P.dram("x1scr", [1024, 2048], F32)
    xs = P.dram("xsscr", [NB * 128, 2048], F32)
    ysd = P.dram("ysscr", [NB * 128, 2048], F32)

    ident = P.sb("ident", [128, 128])
    cb = P.sb("cb", [128, 4])
    colv_sb = P.sb("colv_sb", [128, 32]); sc2p = P.sb("sc2p", [128, 16])
    wr_sb = P.sb("wr_sb", [128, 16 * 72]); br_sb = P.sb("br_sb", [128, 72])
    gates = P.sb("gates", [128, 8 * 64])
    sm = P.sb("sm", [128, 512])
    thr_sb = P.sb("thr_sb", [128, 260])
    thrc = thr_sb[:, 0:1]; Ustr = thr_sb[:, 1:129]; onesm = thr_sb[:, 129:257]; pidx = thr_sb[:, 257:258]
    idxw = P.sb("idxw", [128, 4 * 128], I32); idxf = P.sb("idxf", [128, 128]); idx4 = P.sb("idx4", [128, 4 * 128]); dbe = P.sb("dbe", [128, 128])
    aoh = P.sb("aoh", [128, 8 * 64]); rank = P.sb("rank", [128, 8 * 64]); keyt = P.sb("keyt", [128, 64])[:]
    rt = P.sb("rt", [128, 6 * 64])
    dd = P.sb("dd", [128, 8 * 8])
    gg = P.sb("gg", [128, 8 * 2])
    di = P.sb("di", [128, 8 * 2], I32)
    bef = P.sb("bef", [128, 4]); berow = P.sb("berow", [1, 128]); berowi = P.sb("berowi", [1, 128], I32)
    A = P.sb("arena", [128, 43008])
    psb = [P.ps("ps%d" % i, [128, 512]) for i in range(8)]

    yacc = A[:, 0:16384].rearrange("p (n d) -> p n d", n=8)
    woutb = A[:, 0:16384].bitcast(BF16).rearrange("p (k n) -> p k n", k=16)
    h2T = A[:, 16384:24576].bitcast(BF16).rearrange("p (k t) -> p k t", k=16)
    W0 = 24576
    BC = A[:, 36864:43008].rearrange("p (c d) -> p c d", c=3)
    ycb = [A[:, W0 + i*1024:W0 + (i+1)*1024].bitcast(BF16).rearrange("p (k t) -> p k t", k=16) for i in range(2)]
    xt = A[:, W0 + 2048:W0 + 4096]
    r = A[:, W0 + 4096:W0 + 6144]
    h2f = A[:, W0 + 6144:W0 + 8192].rearrange("p (k t) -> p k t", k=16)
    junk0 = A[:, W0 + 8192:W0 + 10240]
    wgb = [A[:, W0 + i*6144:W0 + i*6144 + 2048].bitcast(BF16).rearrange("p (k n) -> p k n", k=16) for i in range(2)]
    wub = [A[:, W0 + i*6144 + 2048:W0 + i*6144 + 4096].bitcast(BF16).rearrange("p (k n) -> p k n", k=16) for i in range(2)]
    wdb = [A[:, W0 + i*6144 + 4096:W0 + i*6144 + 6144].bitcast(BF16).rearrange("p (c n) -> p c n", c=2) for i in range(2)]
    hidT = A[:, 36864:36864 + 2048].bitcast(BF16).rearrange("p (c t) -> p c t", c=4)
    sgb = [A[:, 36864 + 2048 + i*256:36864 + 2048 + (i+1)*256].bitcast(BF16) for i in range(2)]

    P.dma("sync", out=ident[:], in_=idn)
    P.dma("sync", out=colv_sb[:], in_=colv)
    P.dma("sync", out=wr_sb[:], in_=wr.rearrange("p k n -> p (k n)"))
    P.dma("sync", out=br_sb[:], in_=br)
    P.dma("sync", out=thr_sb[:], in_=thr)
    P.dma("sync", out=BC, in_=bc1)
    P.op("vector", "memset", ap=cb[:, 0:1], constant=1e-5)
    P.op("vector", "tensor_scalar", out=sc2p[:], in0=colv_sb[:, 0:16], scalar1=1.0, scalar2=None, op0=ALU.add)
    P.op("vector", "tensor_scalar", out=BC[:, 0, :], in0=BC[:, 0, :], scalar1=1.0, scalar2=None, op0=ALU.add)
    for kg in range(4):
        P.dma("gpsimd", out=woutb[:, kg*4:(kg+1)*4, :], in_=wout[kg*512:(kg+1)*512, :].rearrange("(k p) n -> p k n", p=128),
              slot="wout")

    def ln_tile(src_r, n, gcol, bcol, dst, junk=None):
        junk = junk if junk is not None else junk0
        c0 = (n % 2) * 16
        s1 = sm[:, c0:c0+1]; s2 = sm[:, c0+1:c0+2]; nm = sm[:, c0+2:c0+3]; msq = sm[:, c0+3:c0+4]
        var = sm[:, c0+4:c0+5]; rstd = sm[:, c0+5:c0+6]; nb = sm[:, c0+6:c0+7]
        P.op("scalar", "activation", out=junk, in_=src_r, func=AF.Identity, accum_out=s1)
        P.op("scalar", "activation", out=junk, in_=src_r, func=AF.Square, accum_out=s2)
        P.op("vector", "tensor_scalar", out=nm, in0=s1, scalar1=-1.0 / D, scalar2=None, op0=ALU.mult)
        P.op("vector", "tensor_tensor", out=msq, in0=nm, in1=nm, op=ALU.mult)
        P.op("vector", "tensor_scalar", out=var, in0=s2, scalar1=1.0 / D, scalar2=msq, op0=ALU.mult, op1=ALU.subtract)
        P.op("scalar", "activation", out=rstd, in_=var, func=AF.Ln, bias=cb[:, 0:1])
        P.op("scalar", "activation", out=rstd, in_=rstd, func=AF.Exp, scale=-0.5)
        P.op("vector", "tensor_tensor", out=nb, in0=nm, in1=rstd, op=ALU.mult)
        P.op("scalar", "activation", out=dst, in_=src_r, func=AF.Identity, scale=rstd, bias=nb)
        P.op("vector", "tensor_tensor", out=dst, in0=dst, in1=BC[:, gcol, :], op=ALU.mult)
        P.op("gpsimd", "tensor_tensor", out=dst, in0=dst, in1=BC[:, bcol, :], op=ALU.add)

    for n in range(8):
        tok = slice(n*128, (n+1)*128)
        yb = ycb[n % 2]
        P.dma("gpsimd", out=yb, in_=ycT[:, :, tok], slot="ycb%d" % (n % 2))
        P.dma("sync", out=xt, in_=xin[tok, :])
        for cg in range(4):
            for k in range(16):
                P.op("tensor", "matmul", out=psb[cg][:, :], lhsT=yb[:, k, :], rhs=woutb[:, k, cg*512:(cg+1)*512],
                     start=(k == 0), stop=(k == 15))
        for cg in range(4):
            cs = slice(cg*512, (cg+1)*512)
            P.op("vector", "tensor_tensor", out=r[:, cs], in0=psb[cg][:, :], in1=BC[:, 0, cs], op=ALU.mult)
        P.op("vector", "scalar_tensor_tensor", out=r, in0=xt, scalar=DN_ALPHA, in1=r, op0=ALU.mult, op1=ALU.add)
        ln_tile(r, n, 1, 2, r)
        P.dma("sync", out=x1s[tok, :], in_=r, slot="x1st")
        for k in range(16):
            pt = psb[4 + (k % 2)][:, (k // 2 % 4)*128:(k // 2 % 4 + 1)*128]
            P.op("tensor", "transpose", out=pt, in_=r[:, k*128:(k+1)*128], identity=ident[:])
            P.op("scalar", "activation", out=h2f[:, k, :], in_=pt, func=AF.Identity, scale=sc2p[:, k:k+1],
                 bias=colv_sb[:, 16 + k:17 + k])
        pr = psb[6][:, 0:72]
        for k in range(16):
            P.op("tensor", "matmul", out=pr, lhsT=h2f[:, k, :], rhs=wr_sb[:, k*72:(k+1)*72], start=(k == 0), stop=(k == 15))
        o = 256 + (n % 2) * 128
        lg = sm[:, o:o+72]; tmp3 = sm[:, 192:256].rearrange("p (g j) -> p g j", g=8)
        c1 = o + 112
        gmax = sm[:, c1:c1+1]; ngmax = sm[:, c1+1:c1+2]; sumg = sm[:, c1+2:c1+3]; nm1 = sm[:, c1+3:c1+4]
        den = sm[:, c1+4:c1+5]; rec = sm[:, c1+5:c1+6]
        ohg = sm[:, o+72:o+80]; lsel = sm[:, o+80:o+88]; top8 = sm[:, o+88:o+96]; mask2 = sm[:, o+96:o+104]
        ee = sm[:, o+104:o+112]
        P.op("vector", "tensor_tensor", out=lg, in0=pr, in1=br_sb[:], op=ALU.add)
        P.op("vector", "tensor_reduce", out=gmax, in_=lg[:, 0:8], axis=AX.X, op=ALU.max)
        P.op("vector", "tensor_scalar", out=ohg, in0=lg[:, 0:8], scalar1=gmax, scalar2=None, op0=ALU.is_ge)
        P.op("vector", "tensor_scalar", out=ngmax, in0=gmax, scalar1=-1.0, scalar2=None, op0=ALU.mult)
        P.op("scalar", "activation", out=top8, in_=lg[:, 0:8], func=AF.Exp, bias=ngmax, accum_out=sumg)
        le3 = lg[:, 8:72].rearrange("p (g j) -> p g j", g=8)
        P.op("vector", "tensor_tensor", out=tmp3, in0=le3, in1=ohg.unsqueeze(2).to_broadcast([128, 8, 8]), op=ALU.mult)
        P.op("vector", "tensor_reduce", out=lsel, in_=sm[:, 192:256].rearrange("p (g j) -> p j g", g=8), axis=AX.X, op=ALU.add)
        P.op("vector", "max", out=top8, in_=lsel)
        P.op("vector", "tensor_scalar", out=mask2, in0=lsel, scalar1=top8[:, 1:2], scalar2=None, op0=ALU.is_ge)
        P.op("vector", "tensor_scalar", out=nm1, in0=top8[:, 0:1], scalar1=-1.0, scalar2=None, op0=ALU.mult)
        P.op("scalar", "activation", out=ee, in_=lsel, func=AF.Exp, bias=nm1)
        P.op("vector", "tensor_tensor", out=ee, in0=ee, in1=mask2, op=ALU.mult)
        P.op("vector", "tensor_reduce", out=den, in_=ee, axis=AX.X, op=ALU.add)
        P.op("vector", "tensor_tensor", out=den, in0=den, in1=sumg, op=ALU.mult)
        P.op("vector", "reciprocal", out=rec, in_=den)
        P.op("vector", "tensor_scalar", out=ee, in0=ee, scalar1=rec, scalar2=None, op0=ALU.mult)
        a3 = aoh[:, n*64:(n+1)*64].rearrange("p (g j) -> p g j", g=8)
        P.op("vector", "tensor_tensor", out=a3, in0=ohg.unsqueeze(2).to_broadcast([128, 8, 8]),
             in1=mask2.unsqueeze(1).to_broadcast([128, 8, 8]), op=ALU.mult)
        g3 = gates[:, n*64:(n+1)*64].rearrange("p (g j) -> p g j", g=8)
        P.op("vector", "tensor_tensor", out=g3, in0=ohg.unsqueeze(2).to_broadcast([128, 8, 8]),
             in1=ee.unsqueeze(1).to_broadcast([128, 8, 8]), op=ALU.mult)

    Cc = rt[:, 0:64]; nbk = rt[:, 64:128]; pend = rt[:, 128:192]; ps1 = rt[:, 192:256]; tmpr = rt[:, 256:320]; ones64 = rt[:, 320:384]
    P.op("vector", "memset", ap=ones64, constant=1.0)
    pc = psb[7][:, 0:64]
    for m in range(8):
        P.op("tensor", "matmul", out=pc, lhsT=onesm, rhs=aoh[:, m*64:(m+1)*64], start=(m == 0), stop=(m == 7))
    P.op("vector", "tensor_copy", out=Cc, in_=pc)
    for n in range(8):
        prk = psb[n % 4][:, 0:64]
        for m in range(n):
            P.op("tensor", "matmul", out=prk, lhsT=onesm, rhs=aoh[:, m*64:(m+1)*64], start=(m == 0), stop=False)
        P.op("tensor", "matmul", out=prk, lhsT=Ustr, rhs=aoh[:, n*64:(n+1)*64], start=(n == 0), stop=True)
        P.op("vector", "tensor_copy", out=rank[:, n*64:(n+1)*64], in_=prk)
    P.op("vector", "tensor_scalar", out=nbk, in0=Cc, scalar1=0.0, scalar2=None, op0=ALU.is_gt)
    for j in range(1, 16):
        P.op("vector", "scalar_tensor_tensor", out=nbk, in0=Cc, scalar=128.0 * j, in1=nbk, op0=ALU.is_gt, op1=ALU.add)
    P.op("vector", "tensor_tensor_scan", out=pend, data0=ones64, data1=nbk, initial=0.0, op0=ALU.mult, op1=ALU.add)
    P.op("vector", "tensor_tensor", out=ps1, in0=pend, in1=nbk, op=ALU.subtract)
    P.op("vector", "tensor_scalar", out=ps1, in0=ps1, scalar1=128.0, scalar2=1.0, op0=ALU.mult, op1=ALU.add)
    P.op("vector", "tensor_scalar", out=tmpr, in0=pend, scalar1=128.0, scalar2=None, op0=ALU.mult)
    P.op("vector", "tensor_scalar", out=tmpr, in0=tmpr, scalar1=thrc, scalar2=None, op0=ALU.is_le)
    P.op("vector", "tensor_reduce", out=bef[:, 0:1], in_=tmpr, axis=AX.X, op=ALU.add)
    P.op("vector", "tensor_scalar", out=bef[:, 0:1], in0=bef[:, 0:1], scalar1=63.0, scalar2=None, op0=ALU.min)
    P.op("vector", "tensor_scalar", out=dbe[:], in0=ident[:], scalar1=bef[:, 0:1], scalar2=None, op0=ALU.mult)
    pbe = psb[6][:, 0:128]
    P.op("tensor", "matmul", out=pbe, lhsT=onesm, rhs=dbe[:], start=True, stop=True)
    P.op("vector", "tensor_scalar", out=idxf[:], in0=pbe, scalar1=128.0, scalar2=pidx, op0=ALU.mult, op1=ALU.add)
    for a in range(4):
        P.op("vector", "tensor_scalar", out=idx4[:, a*128:(a+1)*128], in0=idxf[:], scalar1=4.0, scalar2=float(a), op0=ALU.mult, op1=ALU.add)
    P.op("vector", "tensor_copy", out=idxw[:], in_=idx4[:])
    P.op("vector", "memset", ap=r, constant=0.0)
    for blk in range(NB):
        P.dma("sync", out=xs[blk*128:(blk+1)*128, :], in_=r, slot="xsz")
    for n in range(8):
        tok = slice(n*128, (n+1)*128)
        P.op("vector", "tensor_tensor", out=keyt, in0=rank[:, n*64:(n+1)*64], in1=ps1, op=ALU.add)
        P.op("vector", "tensor_tensor", out=keyt, in0=keyt, in1=aoh[:, n*64:(n+1)*64], op=ALU.mult)
        t8 = dd[:, n*8:(n+1)*8]
        P.op("vector", "max", out=t8, in_=keyt)
        for q in range(2):
            P.op("vector", "tensor_scalar", out=tmpr, in0=keyt, scalar1=t8[:, q:q+1], scalar2=None, op0=ALU.is_equal)
            P.op("vector", "tensor_tensor", out=tmpr, in0=tmpr, in1=gates[:, n*64:(n+1)*64], op=ALU.mult)
            P.op("vector", "tensor_reduce", out=gg[:, n*2+q:n*2+q+1], in_=tmpr, axis=AX.X, op=ALU.add)
        P.op("vector", "tensor_scalar", out=bef[:, 2:4], in0=t8[:, 0:2], scalar1=-1.0, scalar2=None, op0=ALU.add)
        P.op("vector", "tensor_copy", out=di[:, n*2:n*2+2], in_=bef[:, 2:4])
        P.dma("sync", out=xt, in_=x1s[tok, :])
        for q in range(2):
            P.dma("gpsimd", out=xs, in_=xt, indirect={"idx": di[:, n*2+q:n*2+q+1], "mode": "scatter"}, slot="xsc")

    WB = 16384
    wgb = [A[:, i*WB:i*WB + 4096].bitcast(BF16).rearrange("p (k n) -> p k n", k=16) for i in range(2)]
    wub = [A[:, i*WB + 4096:i*WB + 8192].bitcast(BF16).rearrange("p (k n) -> p k n", k=16) for i in range(2)]
    wdb = [A[:, i*WB + 8192:i*WB + 12288].bitcast(BF16).rearrange("p (c n) -> p c n", c=4) for i in range(2)]
    xbT = [A[:, i*WB + 12288:i*WB + 13312].bitcast(BF16).rearrange("p (k t) -> p k t", k=16) for i in range(2)]
    hidb = [A[:, i*WB + 13312:i*WB + 13568].bitcast(BF16) for i in range(2)]
    sgb2 = [A[:, i*WB + 13568:i*WB + 13824].bitcast(BF16) for i in range(2)]
    hidT2 = [A[:, i*WB + 13824:i*WB + 14080].bitcast(BF16).rearrange("p (c t) -> p c t", c=4) for i in range(2)]
    xb = [A[:, 32768 + i*2048:32768 + (i+1)*2048] for i in range(2)]
    ysb = [A[:, 36864 + i*2048:36864 + (i+1)*2048] for i in range(2)]
    identb = P.sb("identb", [128, 128], BF16)
    P.op("vector", "tensor_copy", out=identb[:], in_=ident[:])
    ptT = psb[0][:, :].bitcast(BF16)
    for blk in range(NB):
        i = blk % 2

        for a in range(4):
            ix = idxw[:, a*128 + blk:a*128 + blk + 1]
            P.dma("gpsimd", out=wgb[i][:, a*4:(a+1)*4, :].rearrange("p k n -> p (k n)"), in_=wg,
                  indirect={"idx": ix, "mode": "gather"}, slot="wg%d" % i)
            P.dma("gpsimd", out=wub[i][:, a*4:(a+1)*4, :].rearrange("p k n -> p (k n)"), in_=wu,
                  indirect={"idx": ix, "mode": "gather"}, slot="wu%d" % i)
            P.dma("gpsimd", out=wdb[i][:, a, :], in_=wd, indirect={"idx": ix, "mode": "gather"}, slot="wd%d" % i)
        P.dma("sync", out=xb[i], in_=xs[blk*128:(blk+1)*128, :], slot="xb%d" % i)
        for k in range(16):
            pt = psb[k % 2][:, (k // 2 % 4)*128:(k // 2 % 4 + 1)*128]
            P.op("tensor", "transpose", out=pt, in_=xb[i][:, k*128:(k+1)*128], identity=ident[:])
            P.op("scalar", "activation", out=xbT[i][:, k, :], in_=pt, func=AF.Identity, scale=sc2p[:, k:k+1],
                 bias=colv_sb[:, 16 + k:17 + k])
        pg = psb[2]; pu = psb[3]
        for k in range(16):
            P.op("tensor", "matmul", out=pg[:, :], lhsT=xbT[i][:, k, :], rhs=wgb[i][:, k, :], start=(k == 0), stop=(k == 15))
        for k in range(16):
            P.op("tensor", "matmul", out=pu[:, :], lhsT=xbT[i][:, k, :], rhs=wub[i][:, k, :], start=(k == 0), stop=(k == 15))
        P.op("scalar", "activation", out=sgb2[i], in_=pg[:, :], func=AF.Silu)
        P.op("vector", "tensor_tensor", out=hidb[i], in0=sgb2[i], in1=pu[:, :], op=ALU.mult)
        for c in range(4):
            ptt = ptT[:, c*128:(c+1)*128]
            P.op("tensor", "transpose", out=ptt, in_=hidb[i][:, c*128:(c+1)*128], identity=identb[:])
            P.op("vector" if c % 2 else "scalar", "tensor_copy" if c % 2 else "copy", out=hidT2[i][:, c, :], in_=ptt)
        for cg in range(4):
            for c in range(4):
                P.op("tensor", "matmul", out=psb[4 + cg][:, :], lhsT=hidT2[i][:, c, :], rhs=wdb[i][:, c, cg*512:(cg+1)*512],
                     start=(c == 0), stop=(c == 3))
        for cg in range(4):
            P.op("vector" if cg % 2 else "scalar", "tensor_copy" if cg % 2 else "copy", out=ysb[i][:, cg*512:(cg+1)*512], in_=psb[4 + cg][:, :])
        P.dma("sync", out=ysd[blk*128:(blk+1)*128, :], in_=ysb[i], slot="ysd")

    P.dma("sync", out=BC, in_=bc2)
    P.op("vector", "tensor_scalar", out=BC[:, 0, :], in0=BC[:, 0, :], scalar1=1.0, scalar2=None, op0=ALU.add)
    yh = A[:, 0:2048]; yl = A[:, 2048:4096]; xt3 = A[:, 4096:6144]; r3 = A[:, 6144:8192]; junk3 = A[:, 8192:10240]
    for n in range(8):
        tok = slice(n*128, (n+1)*128)
        P.dma("sync", out=xt3, in_=x1s[tok, :])
        P.dma("gpsimd", out=yh, in_=ysd, indirect={"idx": di[:, n*2:n*2+1], "mode": "gather"}, slot="yh")
        P.dma("gpsimd", out=yl, in_=ysd, indirect={"idx": di[:, n*2+1:n*2+2], "mode": "gather"}, slot="yl")
        P.op("vector", "tensor_scalar", out=yh, in0=yh, scalar1=gg[:, n*2:n*2+1], scalar2=None, op0=ALU.mult)
        P.op("vector", "scalar_tensor_tensor", out=yh, in0=yl, scalar=gg[:, n*2+1:n*2+2], in1=yh, op0=ALU.mult, op1=ALU.add)
        P.op("vector", "tensor_tensor", out=r3, in0=yh, in1=BC[:, 0, :], op=ALU.mult)
        P.op("vector", "scalar_tensor_tensor", out=r3, in0=xt3, scalar=DN_ALPHA, in1=r3, op0=ALU.mult, op1=ALU.add)
        ln_tile(r3, n, 1, 2, r3, junk3)
        P.dma("sync", out=xo[tok, :], in_=r3, slot="xost")
    return P.finish()


def host_inputs_post(inp, mod, layer, ycat, xres):
    wout = inp["hyb_w_out"][0] if layer == 0 else inp["mla_w_out"][0]
    idn = np.eye(128, dtype=np.float32)
    wr = np.concatenate([inp["moe_router_g"][layer], inp["moe_router_e"][layer]], axis=1)
    wr = np.ascontiguousarray(wr.reshape(16, 128, 72).transpose(1, 0, 2))
    brv = np.concatenate([inp["moe_bias_g"][layer], inp["moe_bias_e"][layer]])
    br = np.ascontiguousarray(np.tile(brv[None, :], (128, 1)).astype(np.float32))
    ii = np.arange(128)
    thr = np.zeros((128, 260), np.float32)
    thr[:, 0] = 128.0 * ii
    thr[:, 1:129] = (ii[:, None] < ii[None, :]).astype(np.float32)
    thr[:, 129:257] = 1.0
    thr[:, 257] = ii
    wg_l = np.ascontiguousarray(inp["moe_w_gate"][layer].reshape(64, 16, 128, 512).transpose(0, 2, 1, 3)).reshape(64 * 128 * 4, 2048)
    wu_l = np.ascontiguousarray(inp["moe_w_up"][layer].reshape(64, 16, 128, 512).transpose(0, 2, 1, 3)).reshape(64 * 128 * 4, 2048)
    wd_l = np.ascontiguousarray(inp["moe_w_down"][layer].reshape(64, 4, 128, 2048).transpose(0, 2, 1, 3)).reshape(64 * 128 * 4, 2048)
    maps = []
    for core in range(8):
        b, half = core // 2, core % 2
        ts = slice(half*1024, (half+1)*1024)
        m = mod[layer, b]
        sh1, sc1, g1, sh2, sc2, g2 = (m[i*2048:(i+1)*2048] for i in range(6))
        ycT = np.ascontiguousarray(ycat[b, ts, :].T.reshape(16, 128, 1024).transpose(1, 0, 2))
        bc1 = np.ascontiguousarray(np.tile(np.stack([g1, inp["ln_g"][layer, 0], inp["ln_b"][layer, 0]])[None], (128, 1, 1)).astype(np.float32))
        bc2 = np.ascontiguousarray(np.tile(np.stack([g2, inp["ln_g"][layer, 1], inp["ln_b"][layer, 1]])[None], (128, 1, 1)).astype(np.float32))
        colv = np.ascontiguousarray(np.concatenate([sc2.reshape(16, 128).T, sh2.reshape(16, 128).T], axis=1))
        maps.append({"ycT": ycT, "xin": np.ascontiguousarray(xres[b, ts, :]), "wout": wout, "bc1": bc1, "bc2": bc2,
                     "colv": colv, "wr": wr, "br": br, "wg": wg_l, "wu": wu_l, "wd": wd_l, "idn": idn, "thr": thr})
    return maps


def gather_post(results):
    x = np.zeros((4, 2048, 2048), np.float32)
    for core in range(8):
        b, half = core // 2, core % 2
        x[b, half*1024:(half+1)*1024, :] = results[core]["xo"]
    return x


import numpy as np, math

SCALE = 192.0 ** -0.5
C1 = 6.28125
C2 = 2.0 * math.pi - 6.28125


def build_F():
    nc = bass.Bass("TRN2", target_bir_lowering=False)
    P = Prog(nc)
    xT = nc.dram_tensor("xT", [128, 16, 2048], F32, kind="ExternalInput").ap()
    modc = nc.dram_tensor("modc", [128, 32], F32, kind="ExternalInput").ap()
    win = nc.dram_tensor("win", [2048, 1088], F32, kind="ExternalInput").ap()
    nbc = nc.dram_tensor("nbc", [128, 1024], F32, kind="ExternalInput").ap()
    wuq = nc.dram_tensor("wuq", [512, 1536], F32, kind="ExternalInput").ap()
    wuk = nc.dram_tensor("wuk", [512, 1024], F32, kind="ExternalInput").ap()
    wuv = nc.dram_tensor("wuv", [512, 1024], F32, kind="ExternalInput").ap()
    posb = nc.dram_tensor("posb", [32, 2048], I32, kind="ExternalInput").ap()
    invc = nc.dram_tensor("invc", [32, 1], F32, kind="ExternalInput").ap()
    cst = nc.dram_tensor("cst", [128, 256], F32, kind="ExternalInput").ap()
    yc = nc.dram_tensor("yc", [2048, 1024], F32, kind="ExternalOutput").ap()

    csb = P.sb("csb", [128, 256]); ident = csb[:, 0:128]; cmask = csb[:, 128:256]
    identb = P.sb("identb", [128, 128], BF16)
    cb = P.sb("cb", [128, 4])
    mod_sb = P.sb("mod_sb", [128, 32]); scp1 = P.sb("scp1", [128, 16])
    nbc_sb = P.sb("nbc_sb", [128, 1024])
    sm = P.sb("sm", [128, 64])
    inv_sb = P.sb("inv_sb", [32, 1])
    cosT = P.sb("cosT", [32, 2048]); sinT = P.sb("sinT", [32, 2048])
    ang = P.sb("ang", [32, 512]); tf = P.sb("tf", [32, 512]); ti = P.sb("ti", [32, 512], I32); ta = P.sb("ta", [32, 512])
    kr = [P.sb("kr%d" % i, [32, 2048], BF16) for i in range(2)]
    kp = [P.sb("kp%d" % i, [32, 512]) for i in range(2)]
    cqnT = P.sb("cqnT", [128, 4 * 2048], BF16); ckvnT = P.sb("ckvnT", [128, 4 * 2048], BF16)
    cq3 = cqnT[:, :].rearrange("p (k t) -> p k t", k=4); ckv3 = ckvnT[:, :].rearrange("p (k t) -> p k t", k=4)
    wuqb = [P.sb("wuqb%d" % i, [128, 4 * 192], BF16) for i in range(2)]
    wukb = [P.sb("wukb%d" % i, [128, 4 * 128], BF16) for i in range(2)]
    wuvb = [P.sb("wuvb%d" % i, [128, 4 * 128], BF16) for i in range(2)]
    A = P.sb("arena", [128, 28160])
    posi = A[0:32, 16384:18432].bitcast(I32)
    psb = [P.ps("ps%d" % i, [128, 512]) for i in range(6)]
    ptb = P.ps("ptb", [128, 1024], BF16)
    pvo = P.ps("pvo", [128, 512])
    pbi = [0]

    def bank():
        i = pbi[0]; pbi[0] = (i + 1) % 6
        return psb[i]

    P.dma("sync", out=csb[:], in_=cst)
    P.dma("sync", out=mod_sb[:], in_=modc)
    P.dma("sync", out=nbc_sb[:], in_=nbc)
    P.dma("sync", out=inv_sb[:], in_=invc)
    P.dma("sync", out=posi, in_=posb)
    P.op("vector", "memset", ap=cb[:, 0:1], constant=1e-6)
    P.op("vector", "tensor_copy", out=identb[:], in_=ident)
    P.op("vector", "tensor_scalar", out=scp1[:], in0=mod_sb[:, 0:16], scalar1=1.0, scalar2=None, op0=ALU.add)

    def sin_of(dst, shift, ts):
        a = ta
        P.op("vector", "tensor_copy", out=tf[:], in_=posi[:, ts])
        P.op("vector", "tensor_scalar", out=a[:], in0=tf[:], scalar1=inv_sb[:, 0:1], scalar2=shift, op0=ALU.mult, op1=ALU.add)
        P.op("vector", "tensor_scalar", out=tf[:], in0=a[:], scalar1=1.0 / (2 * math.pi), scalar2=None, op0=ALU.mult)
        P.op("vector", "tensor_copy", out=ti[:], in_=tf[:])
        P.op("vector", "tensor_copy", out=tf[:], in_=ti[:])
        P.op("vector", "scalar_tensor_tensor", out=a[:], in0=tf[:], scalar=-C1, in1=a[:], op0=ALU.mult, op1=ALU.add)
        P.op("vector", "scalar_tensor_tensor", out=a[:], in0=tf[:], scalar=-C2, in1=a[:], op0=ALU.mult, op1=ALU.add)
        P.op("vector", "tensor_scalar", out=tf[:], in0=a[:], scalar1=math.pi, scalar2=-2 * math.pi, op0=ALU.is_gt, op1=ALU.mult)
        P.op("vector", "tensor_tensor", out=a[:], in0=a[:], in1=tf[:], op=ALU.add)
        P.op("vector", "tensor_scalar", out=tf[:], in0=a[:], scalar1=-math.pi, scalar2=2 * math.pi, op0=ALU.is_lt, op1=ALU.mult)
        P.op("vector", "tensor_tensor", out=a[:], in0=a[:], in1=tf[:], op=ALU.add)
        P.op("scalar", "activation", out=dst[:, ts], in_=a[:], func=AF.Sin)
    for t4 in range(4):
        sin_of(sinT, 0.0, slice(t4*512, (t4+1)*512))
        sin_of(cosT, math.pi / 2, slice(t4*512, (t4+1)*512))

    def do_rope(x1, x2, o1, o2, ts):
        P.op("vector", "tensor_tensor", out=tf[:], in0=x1, in1=cosT[:, ts], op=ALU.mult)
        P.op("gpsimd", "tensor_tensor", out=ang[:], in0=x2, in1=sinT[:, ts], op=ALU.mult)
        P.op("vector", "tensor_tensor", out=o1, in0=tf[:], in1=ang[:], op=ALU.subtract)
        P.op("vector", "tensor_tensor", out=tf[:], in0=x1, in1=sinT[:, ts], op=ALU.mult)
        P.op("gpsimd", "tensor_tensor", out=ang[:], in0=x2, in1=cosT[:, ts], op=ALU.mult)
        P.op("vector", "tensor_tensor", out=o2, in0=tf[:], in1=ang[:], op=ALU.add)

    hinT = A[:, 0:16384].bitcast(BF16).rearrange("p (k t) -> p k t", k=16)
    xt = A[:, 16384:18432].rearrange("p (k t) -> p k t", k=16)
    winb = A[:, 18432:18432 + 8704].bitcast(BF16).rearrange("p (k n) -> p k n", k=16)
    P.dma("gpsimd", out=winb, in_=win.rearrange("(k p) n -> p k n", p=128))
    for tt in range(16):
        t0 = tt * 128
        P.dma("sync", out=xt, in_=xT[:, :, t0:t0 + 128])
        for k in range(16):
            P.op("scalar", "activation", out=hinT[:, k, t0:t0 + 128], in_=xt[:, k, :], func=AF.Identity,
                 scale=scp1[:, k:k+1], bias=mod_sb[:, 16 + k:17 + k])
    cn = A[:, 18432 + 8704:18432 + 8704 + 1024]
    for n in range(16):
        tok = slice(n*128, (n+1)*128)
        pc = [bank(), bank()]
        for j in range(2):
            for k in range(16):
                P.op("tensor", "matmul", out=pc[j][:, :], lhsT=hinT[:, k, tok], rhs=winb[:, k, j*512:(j+1)*512],
                     start=(k == 0), stop=(k == 15))
        for j in range(2):
            ss = sm[:, j*4:j*4+1]; rstd = sm[:, j*4+1:j*4+2]
            P.op("scalar", "activation", out=cn[:, j*512:(j+1)*512], in_=pc[j][:, :], func=AF.Square, accum_out=ss)
            P.op("scalar", "activation", out=rstd, in_=ss, func=AF.Ln, scale=1.0 / 512, bias=cb[:, 0:1])
            P.op("scalar", "activation", out=rstd, in_=rstd, func=AF.Exp, scale=-0.5)
            P.op("scalar", "activation", out=cn[:, j*512:(j+1)*512], in_=pc[j][:, :], func=AF.Identity, scale=rstd)
            P.op("vector", "tensor_tensor", out=cn[:, j*512:(j+1)*512], in0=cn[:, j*512:(j+1)*512],
                 in1=nbc_sb[:, j*512:(j+1)*512], op=ALU.mult)
        for j in range(2):
            pt = bank()
            for c in range(4):
                P.op("tensor", "transpose", out=pt[:, c*128:(c+1)*128], in_=cn[:, j*512 + c*128:j*512 + (c+1)*128], identity=ident)
            dst = cq3 if j == 0 else ckv3
            for c in range(4):
                P.op("vector" if c % 2 else "scalar", "tensor_copy" if c % 2 else "copy", out=dst[:, c, tok], in_=pt[:, c*128:(c+1)*128])
    for t4 in range(4):
        ts = slice(t4*512, (t4+1)*512)
        for hf in range(2):
            pk = bank()
            for k in range(16):
                P.op("tensor", "matmul", out=pk[0:32, :], lhsT=winb[:, k, 1024 + hf*32:1024 + (hf+1)*32], rhs=hinT[:, k, ts],
                     start=(k == 0), stop=(k == 15))
            P.op("vector", "tensor_copy", out=kp[hf][:], in_=pk[0:32, :])
        do_rope(kp[0][:], kp[1][:], kr[0][:, ts], kr[1][:, ts], ts)

    o0 = 0
    qTn = A[:, o0:o0 + 1024].bitcast(BF16); o0 += 1024
    kTn = A[:, o0:o0 + 1024].bitcast(BF16); o0 += 1024
    Vh = A[:, o0:o0 + 1024].bitcast(BF16).rearrange("p (n e) -> p n e", n=16); o0 += 1024
    q12 = [A[0:32, o0 + i*2048:o0 + (i+1)*2048] for i in range(2)]; o0 += 4096
    qr = [A[0:32, o0 + i*1024:o0 + (i+1)*1024].bitcast(BF16) for i in range(2)]; o0 += 2048
    Ssb = [A[:, o0 + i*2048:o0 + (i+1)*2048] for i in range(2)]; o0 += 4096
    Pb = [A[:, o0 + i*1024:o0 + (i+1)*1024].bitcast(BF16) for i in range(2)]; o0 += 2048
    PTs = [A[:, o0 + i*64:o0 + (i+1)*64].bitcast(BF16) for i in range(4)]; o0 += 256
    osb = [A[:, o0 + i*128:o0 + (i+1)*128] for i in range(2)]; o0 += 256
    for h in range(8):
        wq3 = wuqb[h % 2][:, :].rearrange("p (k n) -> p k n", k=4)
        wk3 = wukb[h % 2][:, :].rearrange("p (k n) -> p k n", k=4)
        wv3 = wuvb[h % 2][:, :].rearrange("p (k n) -> p k n", k=4)
        P.dma("gpsimd", out=wq3, in_=wuq[:, h*192:(h+1)*192].rearrange("(k p) n -> p k n", p=128))
        P.dma("gpsimd", out=wk3, in_=wuk[:, h*128:(h+1)*128].rearrange("(k p) n -> p k n", p=128))
        P.dma("gpsimd", out=wv3, in_=wuv[:, h*128:(h+1)*128].rearrange("(k p) n -> p k n", p=128))
        for t4 in range(4):
            ts = slice(t4*512, (t4+1)*512)
            pq = bank()
            for kc in range(4):
                P.op("tensor", "matmul", out=pq[:, :], lhsT=wq3[:, kc, 0:128], rhs=cq3[:, kc, ts], start=(kc == 0), stop=(kc == 3))
            P.op("scalar", "copy", out=qTn[:, ts], in_=pq[:, :])
            for hf in range(2):
                p2 = bank()
                for kc in range(4):
                    P.op("tensor", "matmul", out=p2[0:32, :], lhsT=wq3[:, kc, 128 + hf*32:160 + hf*32], rhs=cq3[:, kc, ts],
                         start=(kc == 0), stop=(kc == 3))
                P.op("vector", "tensor_copy", out=q12[hf][:, ts], in_=p2[0:32, :])
            pk = bank()
            for kc in range(4):
                P.op("tensor", "matmul", out=pk[:, :], lhsT=wk3[:, kc, :], rhs=ckv3[:, kc, ts], start=(kc == 0), stop=(kc == 3))
            P.op("scalar", "copy", out=kTn[:, ts], in_=pk[:, :])
        for t4 in range(4):
            ts = slice(t4*512, (t4+1)*512)
            do_rope(q12[0][:, ts], q12[1][:, ts], qr[0][:, ts], qr[1][:, ts], ts)
        for n in range(16):
            pv = bank()
            for kc in range(4):
                P.op("tensor", "matmul", out=pv[:, 0:128], lhsT=ckv3[:, kc, n*128:(n+1)*128], rhs=wv3[:, kc, :],
                     start=(kc == 0), stop=(kc == 3))
            P.op("vector" if n % 2 else "scalar", "tensor_copy" if n % 2 else "copy", out=Vh[:, n, :], in_=pv[:, 0:128])
        for qb in range(16):
            qs = slice(qb*128, (qb+1)*128)
            nk = (qb + 1) * 128
            S = Ssb[qb % 2]; Pm = Pb[qb % 2]
            for j in range((nk + 511) // 512):
                w = min(512, nk - j*512)
                ks = slice(j*512, j*512 + w)
                pscore = bank()
                P.op("tensor", "matmul", out=pscore[:, 0:w], lhsT=qTn[:, qs], rhs=kTn[:, ks], start=True, stop=False)
                P.op("tensor", "matmul", out=pscore[:, 0:w], lhsT=qr[0][:, qs], rhs=kr[0][:, ks], start=False, stop=False)
                P.op("tensor", "matmul", out=pscore[:, 0:w], lhsT=qr[1][:, qs], rhs=kr[1][:, ks], start=False, stop=True)
                P.op("scalar", "activation", out=S[:, ks], in_=pscore[:, 0:w], func=AF.Identity, scale=SCALE)
            P.op("vector", "tensor_tensor", out=S[:, qb*128:nk], in0=S[:, qb*128:nk], in1=cmask, op=ALU.add)
            c0 = 16 + (qb % 2) * 8
            mx = sm[:, c0:c0+1]; nmx = sm[:, c0+1:c0+2]; rsum = sm[:, c0+2:c0+3]; rrec = sm[:, c0+3:c0+4]
            P.op("vector", "tensor_reduce", out=mx, in_=S[:, 0:nk], axis=AX.X, op=ALU.max)
            P.op("vector", "tensor_scalar", out=nmx, in0=mx, scalar1=-1.0, scalar2=None, op0=ALU.mult)
            P.op("scalar", "activation", out=Pm[:, 0:nk], in_=S[:, 0:nk], func=AF.Exp, bias=nmx, accum_out=rsum)
            for kb in range(qb + 1):
                ptt = ptb[:, (kb % 8)*128:(kb % 8 + 1)*128]
                P.op("tensor", "transpose", out=ptt, in_=Pm[:, kb*128:(kb+1)*128], identity=identb[:])
                PT = PTs[kb % 4]
                P.op("vector" if kb % 2 else "scalar", "tensor_copy" if kb % 2 else "copy", out=PT, in_=ptt)
                P.op("tensor", "matmul", out=pvo[:, 0:128], lhsT=PT, rhs=Vh[:, kb, :], start=(kb == 0), stop=(kb == qb))
            P.op("vector", "reciprocal", out=rrec, in_=rsum)
            ob = osb[qb % 2]
            P.op("vector", "tensor_scalar", out=ob, in0=pvo[:, 0:128], scalar1=rrec, scalar2=None, op0=ALU.mult)
            P.dma("sync", out=yc[qs, h*128:(h+1)*128], in_=ob, slot="ycst%d" % (qb % 2))
    assert o0 <= 16384
    return P.finish()


def host_inputs_F(inp, mod, x1):
    ident = np.eye(128, dtype=np.float32)
    i = np.arange(128)
    cmask = np.where(i[:, None] >= i[None, :], 0.0, -1e9).astype(np.float32)
    cst = np.ascontiguousarray(np.concatenate([ident, cmask], axis=1))
    inv = (10000.0 ** (-np.arange(32, dtype=np.float32) * np.float32(2.0 / 64))).astype(np.float32).reshape(32, 1)
    nbc = np.ascontiguousarray(np.tile(np.concatenate([inp["mla_q_norm"][0], inp["mla_kv_norm"][0]])[None, :], (128, 1)).astype(np.float32))
    wuq_f = inp["mla_w_uq"][0].reshape(512, 16, 192)
    wukv_f = inp["mla_w_ukv"][0].reshape(512, 16, 256)
    maps = []
    for core in range(8):
        b, hh = core // 2, core % 2
        hs = slice(hh*8, hh*8 + 8)
        xT = np.ascontiguousarray(x1[b].T.reshape(16, 128, 2048).transpose(1, 0, 2))
        m = mod[1, b]
        sh1 = m[0:2048]; sc1 = m[2048:4096]
        modc = np.ascontiguousarray(np.concatenate([sc1.reshape(16, 128).T, sh1.reshape(16, 128).T], axis=1))
        wuq = np.ascontiguousarray(wuq_f[:, hs, :].reshape(512, 8 * 192))
        wuk = np.ascontiguousarray(wukv_f[:, hs, 0:128].reshape(512, 1024))
        wuv = np.ascontiguousarray(wukv_f[:, hs, 128:256].reshape(512, 1024))
        posb = np.ascontiguousarray(np.tile(inp["positions"][b][None, :], (32, 1)).astype(np.int32))
        maps.append({"xT": xT, "modc": modc, "win": inp["mla_w_in"][0], "nbc": nbc, "wuq": wuq, "wuk": wuk, "wuv": wuv,
                     "posb": posb, "invc": inv, "cst": cst})
    return maps


def gather_F(results):
    ycat = np.zeros((4, 2048, 2048), np.float32)
    for core in range(8):
        b, hh = core // 2, core % 2
        ycat[b, :, hh*1024:(hh+1)*1024] = results[core]["yc"]
    return ycat


def _run(nc, maps):
    return run_bass_kernel_spmd(nc, maps, core_ids=list(range(8))).results


def kernel(**inputs):
    inp = {k: np.asarray(v) for k, v in inputs.items()}
    c = inp["c"]
    cT = np.ascontiguousarray(c.T.reshape(16, 128, 4).transpose(1, 0, 2))
    mapsA = [{"cT": cT, "aw": np.ascontiguousarray(inp["ada_w"][:, :, i*1536:(i+1)*1536]),
              "ab": np.ascontiguousarray(inp["ada_b"][:, i*1536:(i+1)*1536])} for i in range(8)]
    resA = _run(build_modA(), mapsA)
    mod = np.concatenate([r["mod"] for r in resA], axis=-1)
    del mapsA
    ycat0 = gather_B(_run(build_B(), host_inputs_B(inp, mod)))
    x1 = gather_post(_run(build_post(), host_inputs_post(inp, mod, 0, ycat0, inp["x"])))
    ycat1 = gather_F(_run(build_F(), host_inputs_F(inp, mod, x1)))
    out = gather_post(_run(build_post(), host_inputs_post(inp, mod, 1, ycat1, x1)))
    return np.ascontiguousarray(out.astype(np.float32))
```
